# Optimizing a Trainium2 kernel written in Bass

```python
import math
import jax
import jax.numpy as jnp
from jax import lax
import numpy as np

D_MODEL = 1024
BATCH = 4
SEQ = 8192
DEPTH = 1

GRID_W = 64
CTX_LEN = 256

ATTN_HEADS = 8
ATTN_KV_HEADS = 2
ATTN_GROUP = ATTN_HEADS // ATTN_KV_HEADS
ATTN_HEAD_DIM = 64
WINDOW = 128
ATTN_BLOCK = 128
ROPE_BASE = 10000.0
DN_HEADS = 4
DN_HEAD_DIM = 128
DN_CONV = 5
DN_CHUNK = 64
ATTN_Q_W = ATTN_HEADS * ATTN_HEAD_DIM
ATTN_KV_W = ATTN_KV_HEADS * ATTN_HEAD_DIM
DN_W = DN_HEADS * DN_HEAD_DIM
MIX_W = ATTN_Q_W + DN_W
D_IN = ATTN_Q_W + 2 * ATTN_KV_W + 4 * DN_W + 4 * DN_HEADS
IN_SPLITS = (ATTN_Q_W, ATTN_Q_W + ATTN_KV_W, ATTN_Q_W + 2 * ATTN_KV_W,
             ATTN_Q_W + 2 * ATTN_KV_W + 3 * DN_W, ATTN_Q_W + 2 * ATTN_KV_W + 4 * DN_W)
PEER_HEADS = 8
PEER_N_KEYS = 128
PEER_EXPERTS = PEER_N_KEYS ** 2
PEER_KEY_HALF = 128
PEER_TOPK = 16
PEER_BLOCK = 128
DEEPNORM_ALPHA = (2 * DEPTH) ** 0.25
DEEPNORM_BETA = (8 * DEPTH) ** -0.25
LN_EPS = 1e-6
NEG_INF = -1e30

kernel_name = "hybrid_swa_gdn_peer_dit_layer"


def standardize(x):
    xf = x.astype(jnp.float32)
    xc = xf - xf.mean(-1, keepdims=True)
    var = (xc * xc).mean(-1, keepdims=True)
    return (xc * lax.rsqrt(var + LN_EPS)).astype(x.dtype)


def layer_norm(x, g, b):
    return standardize(x) * g + b


def modulate(x, shift, scale):
    return standardize(x) * (1 + scale) + shift


def axial_rope(x, row, col):
    half = x.shape[-1] // 2
    nf = half // 2
    freqs = ROPE_BASE ** (-jnp.arange(nf, dtype=jnp.float32) / nf)

    def rotate(xa, pos):
        ang = pos.astype(jnp.float32)[:, None] * freqs
        cos = jnp.cos(ang)[None, :, None, :].astype(x.dtype)
        sin = jnp.sin(ang)[None, :, None, :].astype(x.dtype)
        x1, x2 = xa[..., :nf], xa[..., nf:]
        return jnp.concatenate([x1 * cos - x2 * sin, x1 * sin + x2 * cos], -1)

    return jnp.concatenate([rotate(x[..., :half], row), rotate(x[..., half:], col)], -1)


def softmax_with_sink(logits, sink):
    snk = jnp.broadcast_to(sink.astype(jnp.float32).reshape(ATTN_KV_HEADS, ATTN_GROUP, 1, 1),
                           logits.shape[:-1] + (1,))
    return jax.nn.softmax(jnp.concatenate([logits, snk], -1), axis=-1)[..., :-1]


def banded_window_attention(q, k, v, k_ctx, v_ctx, sink):
    b, n = q.shape[:2]
    nblk = n // ATTN_BLOCK
    scale = ATTN_HEAD_DIM ** -0.5
    qb = q.reshape(b, nblk, ATTN_BLOCK, ATTN_KV_HEADS, ATTN_GROUP, ATTN_HEAD_DIM)
    pad = ((0, 0), (ATTN_BLOCK, ATTN_BLOCK), (0, 0), (0, 0))
    k_pad = jnp.pad(k, pad)
    v_pad = jnp.pad(v, pad)
    n_loc = 3 * ATTN_BLOCK
    q_off = jnp.arange(ATTN_BLOCK)
    k_off = jnp.arange(n_loc) - ATTN_BLOCK
    in_band = jnp.abs(q_off[:, None] - k_off[None, :]) <= WINDOW

    def one_block(i):
        qi = lax.dynamic_index_in_dim(qb, i, axis=1, keepdims=False)
        ki = lax.dynamic_slice_in_dim(k_pad, i * ATTN_BLOCK, n_loc, axis=1)
        vi = lax.dynamic_slice_in_dim(v_pad, i * ATTN_BLOCK, n_loc, axis=1)
        k_abs = i * ATTN_BLOCK + k_off
        mask = in_band & ((k_abs >= 0) & (k_abs < n))[None, :]
        s_loc = jnp.einsum('bqhgd,bkhd->bhgqk', qi, ki).astype(jnp.float32) * scale
        s_loc = jnp.where(mask, s_loc, NEG_INF)
        s_ctx = jnp.einsum('bqhgd,bkhd->bhgqk', qi, k_ctx).astype(jnp.float32) * scale
        p = softmax_with_sink(jnp.concatenate([s_loc, s_ctx], -1), sink).astype(v.dtype)
        return (jnp.einsum('bhgqk,bkhd->bqhgd', p[..., :n_loc], vi)
                + jnp.einsum('bhgqk,bkhd->bqhgd', p[..., n_loc:], v_ctx))

    o = lax.map(one_block, jnp.arange(nblk))
    return jnp.moveaxis(o, 0, 1).reshape(b, n, ATTN_Q_W)


def context_attention(q, k, v, sink):
    b, m = q.shape[:2]
    qg = q.reshape(b, m, ATTN_KV_HEADS, ATTN_GROUP, ATTN_HEAD_DIM)
    s = jnp.einsum('bqhgd,bkhd->bhgqk', qg, k).astype(jnp.float32) * ATTN_HEAD_DIM ** -0.5
    p = softmax_with_sink(s, sink).astype(v.dtype)
    return jnp.einsum('bhgqk,bkhd->bqhgd', p, v).reshape(b, m, ATTN_Q_W)


def centred_conv(x, w):
    pad = (w.shape[0] - 1) // 2
    return lax.conv_general_dilated(x, w[:, None, :], window_strides=(1,), padding=[(pad, pad)],
                                    dimension_numbers=('NWC', 'WIO', 'NWC'),
                                    feature_group_count=x.shape[-1])


def l2norm(x):
    xf = x.astype(jnp.float32)
    return xf * lax.rsqrt(jnp.sum(xf * xf, -1, keepdims=True) + LN_EPS)


def gdn_inputs(qkv, gb, conv_w, a_log, dt_bias):
    b, n, _ = qkv.shape
    qkv = jax.nn.silu(centred_conv(qkv, conv_w))
    q, k, v = jnp.split(qkv, 3, axis=-1)
    q = l2norm(q.reshape(b, n, DN_HEADS, DN_HEAD_DIM)) * DN_HEAD_DIM ** -0.5
    k = l2norm(k.reshape(b, n, DN_HEADS, DN_HEAD_DIM))
    v = v.reshape(b, n, DN_HEADS, DN_HEAD_DIM)
    gb = gb.astype(jnp.float32)
    beta = jax.nn.sigmoid(gb[..., :2 * DN_HEADS]).reshape(b, n, 2, DN_HEADS)
    a = gb[..., 2 * DN_HEADS:].reshape(b, n, 2, DN_HEADS)
    g = -jnp.exp(a_log.astype(jnp.float32)) * jax.nn.softplus(a + dt_bias.astype(jnp.float32))
    return q, k, v, g, beta


def gated_delta_rule(q, k, v, g, beta, s0):
    b, n, h, dk = q.shape
    dv = v.shape[-1]
    nc = n // DN_CHUNK

    def chunks(t):
        t = t.astype(jnp.float32).reshape((b, nc, DN_CHUNK) + t.shape[2:])
        return jnp.swapaxes(t, 2, 3)

    q, k, v, g, beta = chunks(q), chunks(k), chunks(v), chunks(g), chunks(beta)
    gam = jnp.cumsum(g, axis=-1)
    causal = jnp.tril(jnp.ones((DN_CHUNK, DN_CHUNK), bool))
    strict = jnp.tril(jnp.ones((DN_CHUNK, DN_CHUNK), bool), -1)
    diff = gam[..., :, None] - gam[..., None, :]
    decay = jnp.where(causal, jnp.exp(jnp.where(causal, diff, 0.0)), 0.0)
    kb = k * beta[..., None]
    a_mat = jnp.where(strict, jnp.einsum('bnhid,bnhjd->bnhij', kb, k) * decay, 0.0)
    rhs = jnp.concatenate([v * beta[..., None], kb * jnp.exp(gam)[..., None]], -1)
    sol = lax.linalg.triangular_solve(a_mat, rhs, left_side=True, lower=True, unit_diagonal=True)
    u_c, w_c = sol[..., :dv], sol[..., dv:]
    qk = jnp.einsum('bnhid,bnhjd->bnhij', q, k) * decay
    q_dec = q * jnp.exp(gam)[..., None]
    k_dec = k * jnp.exp(gam[..., -1:] - gam)[..., None]
    g_tot = jnp.exp(gam[..., -1])

    def step(state, inp):
        qk_i, qd_i, kd_i, u_i, w_i, gt_i = inp
        v_new = u_i - jnp.einsum('bhid,bhde->bhie', w_i, state)
        o = jnp.einsum('bhid,bhde->bhie', qd_i, state) + jnp.einsum('bhij,bhje->bhie', qk_i, v_new)
        state = state * gt_i[..., None, None] + jnp.einsum('bhid,bhie->bhde', kd_i, v_new)
        return state, o

    xs = tuple(jnp.moveaxis(t, 1, 0) for t in (qk, q_dec, k_dec, u_c, w_c, g_tot))
    s_final, o = lax.scan(step, s0, xs)
    o = jnp.swapaxes(jnp.moveaxis(o, 0, 1), 2, 3).reshape(b, n, h, dv)
    return o, s_final


def bidir_delta(q, k, v, g, beta, s0_f, s0_b):
    o_f, s_f = gated_delta_rule(q, k, v, g[:, :, 0], beta[:, :, 0], s0_f)
    flip = lambda t: jnp.flip(t, axis=1)
    o_b, s_b = gated_delta_rule(flip(q), flip(k), flip(v), flip(g[:, :, 1]), flip(beta[:, :, 1]), s0_b)
    return o_f + flip(o_b), s_f, s_b


def gated_rmsnorm(o, z, w):
    b, n = z.shape[:2]
    y = o * lax.rsqrt(jnp.mean(o * o, -1, keepdims=True) + LN_EPS) * w.astype(jnp.float32)
    return y.astype(z.dtype).reshape(b, n, DN_W) * jax.nn.silu(z)


def mixing_sublayer(h, hc, w_in, conv_w, a_log, dt_bias, sink, dn_norm_w, w_out, with_ctx_out):
    b, n, _ = h.shape
    m = hc.shape[1]
    rows = n // GRID_W
    row = jnp.broadcast_to(jnp.arange(rows)[:, None], (rows, GRID_W)).reshape(n)
    col = jnp.broadcast_to(jnp.arange(GRID_W)[None, :], (rows, GRID_W)).reshape(n)
    qa, ka, va, qkv_d, z, gb = jnp.split(h @ w_in, IN_SPLITS, axis=-1)
    qa_c, ka_c, va_c, qkv_dc, z_c, gb_c = jnp.split(hc @ w_in, IN_SPLITS, axis=-1)
    qa = axial_rope(qa.reshape(b, n, ATTN_HEADS, ATTN_HEAD_DIM), row, col)
    ka = axial_rope(ka.reshape(b, n, ATTN_KV_HEADS, ATTN_HEAD_DIM), row, col)
    va = va.reshape(b, n, ATTN_KV_HEADS, ATTN_HEAD_DIM)
    ka_c = ka_c.reshape(b, m, ATTN_KV_HEADS, ATTN_HEAD_DIM)
    va_c = va_c.reshape(b, m, ATTN_KV_HEADS, ATTN_HEAD_DIM)
    attn = banded_window_attention(qa, ka, va, ka_c, va_c, sink)
    qd_c, kd_c, vd_c, g_c, beta_c = gdn_inputs(qkv_dc, gb_c, conv_w, a_log, dt_bias)
    s0 = jnp.zeros((b, DN_HEADS, DN_HEAD_DIM, DN_HEAD_DIM), jnp.float32)
    o_c, s_f, s_b = bidir_delta(qd_c, kd_c, vd_c, g_c, beta_c, s0, s0)
    qd, kd, vd, g, beta = gdn_inputs(qkv_d, gb, conv_w, a_log, dt_bias)
    o, _, _ = bidir_delta(qd, kd, vd, g, beta, s_f, s_b)
    y = jnp.concatenate([attn, gated_rmsnorm(o, z, dn_norm_w)], -1) @ w_out
    if not with_ctx_out:
        return y, None
    attn_c = context_attention(qa_c.reshape(b, m, ATTN_HEADS, ATTN_HEAD_DIM), ka_c, va_c, sink)
    y_c = jnp.concatenate([attn_c, gated_rmsnorm(o_c, z_c, dn_norm_w)], -1) @ w_out
    return y, y_c


def peer(h, w_query, sub_keys, u_tab, v_tab):
    b, n, d = h.shape
    tokens = h.reshape(-1, PEER_BLOCK, d)

    def one_block(xt):
        q = (xt @ w_query).reshape(PEER_BLOCK, PEER_HEADS, 2, PEER_KEY_HALF)
        s = jnp.einsum('phxd,xkd->phxk', q, sub_keys).astype(jnp.float32)
        s_top, i_top = lax.top_k(s, PEER_TOPK)
        cand = (s_top[:, :, 0, :, None] + s_top[:, :, 1, None, :]).reshape(
            PEER_BLOCK, PEER_HEADS, PEER_TOPK * PEER_TOPK)
        best, pos = lax.top_k(cand, PEER_TOPK)
        i1 = jnp.take_along_axis(i_top[:, :, 0], pos // PEER_TOPK, axis=-1)
        i2 = jnp.take_along_axis(i_top[:, :, 1], pos % PEER_TOPK, axis=-1)
        expert = i1 * PEER_N_KEYS + i2
        gate = jax.nn.softmax(best, axis=-1).astype(xt.dtype)
        act = jax.nn.gelu(jnp.einsum('phkd,pd->phk', u_tab[expert], xt), approximate=False)
        return jnp.einsum('phk,phkd->pd', gate * act, v_tab[expert])

    return lax.map(one_block, tokens).reshape(b, n, d)


def setup_inputs(seed: int = 0) -> dict:
    key = jax.random.key(seed)
    ks = jax.random.split(key, 22)
    d = D_MODEL

    def nrm(k, shape, s):
        return jax.random.normal(k, shape, jnp.float32) * s

    dt = jnp.exp(jax.random.uniform(ks[9], (DEPTH, 2, DN_HEADS), jnp.float32,
                                    minval=math.log(1e-3), maxval=math.log(1e-1)))
    return {
        "x": nrm(ks[0], (BATCH, SEQ, d), 1.0),
        "c": nrm(ks[1], (BATCH, d), 1.0),
        "ctx": nrm(ks[2], (BATCH, CTX_LEN, d), 1.0),
        "c_ctx": nrm(ks[3], (d,), 1.0),
        "w_ada": nrm(ks[4], (DEPTH, d, 6 * d), d ** -0.5),
        "b_ada": nrm(ks[5], (DEPTH, 6 * d), 0.02),
        "w_in": nrm(ks[6], (DEPTH, d, D_IN), d ** -0.5),
        "conv_w": nrm(ks[7], (DEPTH, DN_CONV, 3 * DN_W), DN_CONV ** -0.5),
        "a_log": jnp.log(jax.random.uniform(ks[8], (DEPTH, 2, DN_HEADS), jnp.float32,
                                            minval=1.0, maxval=16.0)),
        "dt_bias": dt + jnp.log(-jnp.expm1(-dt)),
        "sink": nrm(ks[10], (DEPTH, ATTN_HEADS), 0.5),
        "dn_norm_w": 1.0 + nrm(ks[11], (DEPTH, DN_HEAD_DIM), 0.02),
        "w_out": nrm(ks[12], (DEPTH, MIX_W, d), MIX_W ** -0.5 * DEEPNORM_BETA),
        "ln1_g": 1.0 + nrm(ks[13], (DEPTH, d), 0.02),
        "ln1_b": nrm(ks[14], (DEPTH, d), 0.02),
        "peer_wq": nrm(ks[15], (DEPTH, d, PEER_HEADS * 2 * PEER_KEY_HALF), d ** -0.5),
        "peer_sub_keys": nrm(ks[16], (DEPTH, 2, PEER_N_KEYS, PEER_KEY_HALF), PEER_KEY_HALF ** -0.5),
        "peer_u": nrm(ks[17], (DEPTH, PEER_EXPERTS, d), d ** -0.5),
        "peer_v": nrm(ks[18], (DEPTH, PEER_EXPERTS, d), DEEPNORM_BETA),
        "ln2_g": 1.0 + nrm(ks[19], (DEPTH, d), 0.02),
        "ln2_b": nrm(ks[20], (DEPTH, d), 0.02),
    }


def reference(x, c, ctx, c_ctx, w_ada, b_ada, w_in, conv_w, a_log, dt_bias, sink, dn_norm_w, w_out,
              ln1_g, ln1_b, peer_wq, peer_sub_keys, peer_u, peer_v, ln2_g, ln2_b):
    for l in range(DEPTH):
        last = l == DEPTH - 1
        mod = jax.nn.silu(c) @ w_ada[l] + b_ada[l]
        mod_c = jax.nn.silu(c_ctx) @ w_ada[l] + b_ada[l]
        sh1, sc1, gt1, sh2, sc2, gt2 = [t[:, None, :] for t in jnp.split(mod, 6, axis=-1)]
        csh1, csc1, cgt1, csh2, csc2, cgt2 = jnp.split(mod_c, 6, axis=-1)
        h = modulate(x, sh1, sc1)
        hc = modulate(ctx, csh1, csc1)
        y, y_c = mixing_sublayer(h, hc, w_in[l], conv_w[l], a_log[l], dt_bias[l], sink[l],
                                 dn_norm_w[l], w_out[l], not last)
        x = layer_norm(DEEPNORM_ALPHA * x + gt1 * y, ln1_g[l], ln1_b[l])
        h = modulate(x, sh2, sc2)
        x = layer_norm(DEEPNORM_ALPHA * x + gt2 * peer(h, peer_wq[l], peer_sub_keys[l], peer_u[l], peer_v[l]),
                       ln2_g[l], ln2_b[l])
        if not last:
            ctx = layer_norm(DEEPNORM_ALPHA * ctx + cgt1 * y_c, ln1_g[l], ln1_b[l])
            hc = modulate(ctx, csh2, csc2)
            ctx = layer_norm(DEEPNORM_ALPHA * ctx + cgt2 * peer(hc, peer_wq[l], peer_sub_keys[l], peer_u[l], peer_v[l]),
                             ln2_g[l], ln2_b[l])
    return x
```

```python
import numpy as np
from contextlib import ExitStack
import concourse.bass as bass
import concourse.mybir as mybir
from concourse.bass_utils import run_bass_kernel_spmd

F32 = mybir.dt.float32
BF16 = mybir.dt.bfloat16
I32 = mybir.dt.int32
U32 = mybir.dt.uint32
AF = mybir.ActivationFunctionType
ALU = mybir.AluOpType
AX = mybir.AxisListType

ENGS = ['pe', 'act', 'dve', 'pool', 'sp']

D = 1024
QA, QAP, KA, KAP, DQ, DK, DV, ZC, VA, GB, WEXT = 0, 512, 1024, 1152, 1280, 1792, 2304, 2816, 3328, 3456, 3472
EPS = 1e-6
ALPHA = 2.0 ** 0.25
BIG = 30000.0


class Prog:
    def __init__(self, nc, es):
        self.nc = nc
        self.es = es
        self.recs = {e: [] for e in ENGS}
        self.cnt = {e: 0 for e in ENGS}
        self.known = {e: {} for e in ENGS}
        self.state = {}
        self.esem = {e: es.enter_context(nc.semaphore('s_' + e)) for e in ENGS if e != 'sp'}
        self.dsem = {}
        self.dcnt = {}
        self.tokinfo = {}
        self.nwaits = 0
        self.free_sems = []
        self.all_sems = []
        self.semcnt = {}
        self.uniq = 0

    def dma_sem(self, name):
        if name not in self.dsem:
            if self.free_sems:
                sem = self.free_sems.pop()
            else:
                sem = self.es.enter_context(self.nc.semaphore('d_%d' % len(self.all_sems)))
                self.all_sems.append(sem)
                self.semcnt[sem] = 0
            self.dsem[name] = sem
        return name

    def op(self, eng, fn, reads=(), writes=(), dma=None):
        waits = {}
        kn = self.known[eng]
        own = self.esem.get(eng)

        def need(tok):
            sem, val = tok
            if eng == 'pe' and sem is own:
                return
            if kn.get(sem, 0) >= val:
                return
            if waits.get(sem, 0) < val:
                waits[sem] = val

        for k in reads:
            st = self.state.get(k)
            if st is not None and st[0] is not None:
                need(st[0])
        for k in writes:
            st = self.state.get(k)
            if st is not None:
                if st[0] is not None:
                    need(st[0])
                for r in st[1]:
                    need(r)
        for sem, val in waits.items():
            kn[sem] = val
            info = self.tokinfo.get((sem, val))
            if info:
                for s2, v2 in info.items():
                    if kn.get(s2, 0) < v2:
                        kn[s2] = v2
        if dma is None:
            self.cnt[eng] += 1
            tok = (own, self.cnt[eng])
            inc = (own, 1)
        else:
            if dma == '*':
                self.uniq += 1
                dma = '*%d' % self.uniq
            self.dma_sem(dma)
            sem = self.dsem[dma]
            self.semcnt[sem] += 16
            tok = (sem, self.semcnt[sem])
            inc = (sem, 16)
        self.tokinfo[tok] = dict(kn)
        for k in reads:
            st = self.state.get(k)
            if st is None:
                self.state[k] = [None, [tok]]
            else:
                st[1].append(tok)
        for k in writes:
            self.state[k] = [tok, []]
        self.nwaits += len(waits)
        self.recs[eng].append((list(waits.items()), fn, inc))
        return tok

    def barrier(self):
        toks = []
        for e in ENGS:
            if e != 'sp' and self.cnt[e] > 0:
                toks.append((self.esem[e], self.cnt[e]))
        for sem in self.all_sems:
            if self.semcnt[sem] > 0:
                toks.append((sem, self.semcnt[sem]))
        for e in ENGS:
            kn = self.known[e]
            w = []
            for sem, val in toks:
                if kn.get(sem, 0) < val:
                    kn[sem] = val
                    w.append((sem, val))
            if w:
                self.recs[e].append((w, None, None))
        self.state = {}
        self.tokinfo = {}
        self.free_sems = list(self.all_sems)
        self.dsem = {}

    def emit(self):
        nc = self.nc
        self.barrier()
        with nc.Block() as block:
            def run(name):
                def f(e):
                    for waits, fn, inc in self.recs[name]:
                        for sem, val in waits:
                            e.wait_ge(sem, val)
                        if fn is not None:
                            ins = fn(e)
                            ins.then_inc(inc[0], inc[1])
                return f
            block.tensor(run('pe'))
            block.scalar(run('act'))
            block.vector(run('dve'))
            block.gpsimd(run('pool'))
            block.sync(run('sp'))


def MM(out, lhsT, rhs, st, sp):
    return lambda e: e.matmul(out, lhsT=lhsT, rhs=rhs, start=st, stop=sp)


def TR(out, in_, ident):
    return lambda e: e.transpose(out=out, in_=in_, identity=ident)


def ACT(out, in_, func, scale=1.0, bias=None, accum=None):
    kw = {}
    if bias is not None:
        kw['bias'] = bias
    if accum is not None:
        kw['accum_out'] = accum
    return lambda e: e.activation(out=out, in_=in_, func=func, scale=scale, **kw)


def ACP(out, in_):
    return lambda e: e.copy(out=out, in_=in_)


def TT(out, a, b, op):
    return lambda e: e.tensor_tensor(out=out, in0=a, in1=b, op=op)


def TS(out, a, s1, op0, s2=None, op1=None, accum=None):
    kw = {}
    if op1 is not None:
        kw['op1'] = op1
    if accum is not None:
        kw['accum_out'] = accum
    return lambda e: e.tensor_scalar(out=out, in0=a, scalar1=s1, scalar2=s2, op0=op0, **kw)


def STT(out, a, s, b, op0, op1, accum=None):
    kw = {}
    if accum is not None:
        kw['accum_out'] = accum
    return lambda e: e.scalar_tensor_tensor(out=out, in0=a, scalar=s, in1=b, op0=op0, op1=op1, **kw)


def CP(out, in_):
    return lambda e: e.tensor_copy(out=out, in_=in_)


def MEMSET(out, v):
    return lambda e: e.memset(out, v)


def DMA(out, in_):
    return lambda e: e.dma_start(out=out, in_=in_)


def RECIP(out, in_):
    return lambda e: e.reciprocal(out=out, in_=in_)


class Builder:
    def __init__(self, NT, dbg=(), stop_after=None):
        self.NT = NT
        self.dbg = set(dbg)
        self.stop_after = stop_after
        self.nc = bass.Bass("TRN2", target_bir_lowering=False)
        self.NL = 2 * NT
        self.NG = 256 + self.NL * 128
        self.NO = NT * 128

    def din(self, name, shape, dt=F32):
        return self.nc.dram_tensor(name, list(shape), dt, kind="ExternalInput").ap()

    def dout(self, name, shape, dt=F32):
        return self.nc.dram_tensor(name, list(shape), dt, kind="ExternalOutput").ap()

    def dscr(self, name, shape, dt=F32):
        kind = "ExternalOutput" if self.dbg else "Internal"
        return self.nc.dram_tensor(name, list(shape), dt, kind=kind).ap()

    def build(self):
        nc = self.nc
        NT, NL, NG, NO = self.NT, self.NL, self.NG, self.NO
        I = {}
        I['x'] = self.din('x', [NL * 128, D])
        I['ctx'] = self.din('ctx', [256, D])
        I['cvec'] = self.din('cvec', [128, 16])
        I['w_ada'] = self.din('w_ada', [D, 6 * D])
        I['b_ada'] = self.din('b_ada', [1, 6 * D])
        I['w_in'] = self.din('w_in', [D, WEXT])
        I['cw'] = self.din('cw', [128, 12, 5])
        I['adt'] = self.din('adt', [1, 16])
        I['sink'] = self.din('sink', [1, 8])
        I['wnorm'] = self.din('wnorm', [128, 512])
        I['w_out'] = self.din('w_out', [D, D])
        I['lnp'] = self.din('lnp', [128, 4 * D])
        I['wq'] = self.din('wq', [D, 2048])
        I['skT'] = self.din('skT', [128, 2, 128])
        I['peer_u'] = self.din('peer_u', [16384, D])
        I['peer_v'] = self.din('peer_v', [16384, D])
        I['ropeC'] = self.din('ropeC', [64, (NT + 1) * 128])
        I['ropeS'] = self.din('ropeS', [64, (NT + 1) * 128])
        I['cf'] = self.din('cf', [128, 1280])
        I['cb'] = self.din('cb', [128, 512], BF16)
        self.I = I
        self.out = self.dout('out', [NO, D])
        S = {}
        S['kT'] = self.dscr('kT_s', [4, 128, NG])
        S['qT'] = self.dscr('qT_s', [4, 128, NO])
        S['ktok'] = self.dscr('ktok_s', [NG, 512])
        S['vtok'] = self.dscr('vtok_s', [NG, 512])
        S['gate'] = self.dscr('gate_s', [NG, 24])
        S['zs'] = self.dscr('zs_s', [NO, 512])
        S['attnT'] = self.dscr('attnT_s', [64, 8, NO], BF16)
        S['oA'] = self.dscr('oA_s', [NO, 512])
        S['x1'] = self.dscr('x1_s', [NO, D])
        S['oB'] = self.dscr('oB_s', [NO, 512])
        S['uv'] = self.dscr('uv_s', [16384, 2 * D], BF16)
        self.dbg_state = {}
        if 'state' in self.dbg:
            self.dbg_state = {t: self.dout('dbgS_' + t, [128, 512]) for t in ('ga_', 'gb_')}
        self.S = S
        self.dbg_out = {}
        with ExitStack() as es:
            self.es = es
            self.P = Prog(nc, es)
            self.phase0()
            if self.stop_after not in ('p0', 'p0a', 'p0b', 'p0c', 'p0d', 'p0e'):
                self.phaseA()
                if self.stop_after not in ('A',):
                    self.phaseG()
                    if self.stop_after not in ('GA', 'G'):
                        self.phaseM()
                        if self.stop_after not in ('M',):
                            self.phaseC()
            self.P.emit()
        return nc

    def sb(self, name, shape, dt=F32, es=None):
        return (es or self.es).enter_context(self.nc.sbuf_tensor('sb_' + name, list(shape), dt))

    def pst(self, name, shape, dt=F32, es=None):
        return (es or self.es).enter_context(self.nc.psum_tensor('pp_' + name, list(shape), dt))

    def psum(self):
        lim = getattr(self, 'ps_lim', len(self.psf))
        i = self.ps_next % lim
        self.ps_next = (i + 1) % lim
        return self.psf[i], 'ps%d' % i

    def dump(self, name, ap_sb, shape, reads, dt=F32):
        if name not in self.dbg_out:
            self.dbg_out[name] = self.dout('dbg_' + name, shape, dt)
        return self.dbg_out[name]

    def phase0(self):
        P, I, nc = self.P, self.I, self.nc
        sb = self.sb
        self.psf = [self.pst('psf%d' % i, [128, 512]) for i in range(7)]
        self.psb = self.pst('psb', [128, 1024], BF16)
        self.ps_next = 0
        self.cf = sb('cf', [128, 1280])
        self.cb = sb('cb', [128, 512], BF16)
        P.op('sp', DMA(self.cf[:], I['cf'][:, :]), writes=['cf'], dma='*')
        P.op('sp', DMA(self.cb[:], I['cb'][:, :]), writes=['cb'], dma='*')
        self.ident_f = self.cf[:, 0:128]
        self.ones_f = self.cf[:, 128:256]
        self.ident_b = self.cb[:, 0:128]
        self.maskL = self.cb[:, 128:256]
        self.maskR = self.cb[:, 256:384]
        self.ones_b = self.cb[:, 384:512]
        self.epsc = sb('epsc', [128, 4])
        P.op('pool', MEMSET(self.epsc[:, 0:1], EPS), writes=['epsc0'])
        P.op('pool', MEMSET(self.epsc[:, 1:2], 128 * EPS), writes=['epsc1'])
        P.op('pool', MEMSET(self.epsc[:, 2:3], 1.0), writes=['epsc2'])
        self.EPSK = ['epsc0', 'epsc1', 'epsc2']
        if self.stop_after == 'p0a':
            return
        self.modb = sb('modb', [128, 6 * D])
        self.esA = ExitStack()
        sbA = lambda name, shape, dt=F32: self.sb(name, shape, dt, es=self.esA)
        self.modc = sbA('modc', [128, 2 * D])
        self.w_in = sbA('w_in_sb', [128, 8, WEXT], BF16)
        self.cw = sbA('cw', [128, 12, 5])
        adt = sbA('adt', [128, 16])
        self.nexp = sbA('nexp', [128, 8])
        sk = sbA('sk', [64, 8])
        self.esink = sbA('esink', [64, 8, 128])
        self.wnbc = sbA('wnbc', [128, 4, 128])
        es2 = ExitStack()
        sb2 = lambda name, shape, dt=F32: self.sb(name, shape, dt, es=es2)
        cv = sb2('cv', [128, 16])
        P.op('sp', DMA(cv[:], I['cvec'][:, :]), writes=['cv'], dma='*')
        cs_ = sb2('cs_', [128, 16])
        P.op('act', ACT(cs_[:], cv[:], AF.Silu), reads=['cv'], writes=['cs_'])
        crep = sb2('crep', [128, 16, 128])
        P.op('dve', CP(crep[:], cs_[:, :].unsqueeze(2).to_broadcast([128, 16, 128])), reads=['cs_'], writes=['crep'])
        bada = sb2('bada', [1, 6 * D])
        P.op('sp', DMA(bada[:], I['b_ada'][:, :]), writes=['bada'], dma='*')
        wst = [self.sb('wst%d' % i, [128, 8, 512], es=es2) for i in range(2)]
        wa = I['w_ada'].rearrange("(kc p) n -> p kc n", p=128)
        for n in range(12):
            s = n % 2
            P.op('sp', DMA(wst[s][:], wa[:, :, n * 512:(n + 1) * 512]), writes=['wst%d' % s], dma='wst%d' % s)
            variants = [(0, self.modb)] + ([(8, self.modc)] if n < 4 else [])
            for off, dst in variants:
                ps, pk = self.psum()
                for kc in range(8):
                    P.op('pe', MM(ps[:, :], crep[:, off + kc, :], wst[s][:, kc, :], kc == 0, False),
                         reads=['crep', 'wst%d' % s], writes=[pk])
                P.op('pe', MM(ps[:, :], self.ones_f[0:1, :], bada[0:1, n * 512:(n + 1) * 512], False, True),
                     reads=['cf', 'bada'], writes=[pk])
                dk = ('mod', id(dst), n)
                if n in (2, 3, 8, 9):
                    P.op('dve', TS(dst[:, n * 512:(n + 1) * 512], ps[:, :], 1.0, ALU.add), reads=[pk], writes=[dk])
                else:
                    P.op('act', ACP(dst[:, n * 512:(n + 1) * 512], ps[:, :]), reads=[pk], writes=[dk])
        self.MODK = [('mod', id(self.modb), n) for n in range(12)]
        self.MODCK = [('mod', id(self.modc), n) for n in range(4)]
        P.barrier()
        es2.close()
        if self.stop_after == 'p0b':
            return
        for kc in range(8):
            for (a, b) in ((0, 1736), (1736, WEXT)):
                P.op('pool', DMA(self.w_in[:, kc, a:b], I['w_in'][kc * 128:(kc + 1) * 128, a:b]),
                     writes=[('w_in', kc, a)], dma='w_in')
        self.WINK = [('w_in', kc, a) for kc in range(8) for a in (0, 1736)]
        if self.stop_after == 'p0c':
            return
        P.op('sp', DMA(self.cw[:], I['cw'][:, :, :]), writes=['cw'], dma='*')
        P.op('sp', DMA(adt[:], I['adt'].partition_broadcast(128)), writes=['adt'], dma='*')
        P.op('act', ACT(self.nexp[:], adt[:, 0:8], AF.Exp), reads=['adt'], writes=['nexp'])
        P.op('dve', TS(self.nexp[:], self.nexp[:], -1.0, ALU.mult), reads=['nexp'], writes=['nexp'])
        self.dtb = adt[:, 8:16]
        if self.stop_after == 'p0d':
            return
        P.op('sp', DMA(sk[:], I['sink'].partition_broadcast(64)), writes=['sk'], dma='*')
        P.op('act', ACT(sk[:], sk[:], AF.Exp), reads=['sk'], writes=['sk'])
        P.op('dve', CP(self.esink[:], sk[:, :].unsqueeze(2).to_broadcast([64, 8, 128])), reads=['sk'], writes=['esink'])
        if self.stop_after == 'p0e':
            return
        P.op('sp', DMA(self.wnbc[:].rearrange("p a b -> p (a b)"), I['wnorm'][:, :]), writes=['wnbc'], dma='*')

    def phaseA(self):
        P, I, S, nc = self.P, self.I, self.S, self.nc
        NT, NL = self.NT, self.NL
        es = ExitStack()
        sb = lambda name, shape, dt=F32: self.sb(name, shape, dt, es=es)
        xt = [sb('xt%d' % i, [128, D]) for i in range(2)]
        xn = sb('xn', [128, D])
        hb = [sb('hb%d' % i, [128, D], BF16) for i in range(2)]
        hT = [sb('hT%d' % i, [128, 8, 128], BF16) for i in range(2)]
        st6 = sb('st6', [128, 2, 6])
        mv = sb('mv', [128, 2])
        rstd = sb('rstd', [128, 1])
        pad = [sb('pad%d' % i, [128, 12, 132]) for i in range(3)]
        cs = [sb('cs%d' % i, [128, 12, 128]) for i in range(2)]
        sq = sb('sq', [128, 8, 128])
        rn = sb('rn', [128, 8, 128])
        rC = [sb('rC%d' % i, [64, 128]) for i in range(2)]
        rS = [sb('rS%d' % i, [64, 128]) for i in range(2)]
        rt1 = sb('rt1', [64, 2, 128])
        rt2 = sb('rt2', [64, 2, 128])
        kTr = [sb('kTr%d' % i, [64, 2, 128], BF16) for i in range(4)]
        vr = [sb('vr%d' % i, [128, 2, 64], BF16) for i in range(4)]
        qTr = [sb('qTr%d' % i, [64, 8, 128], BF16) for i in range(2)]
        kTc = sb('kTc', [64, 2, 2, 128], BF16)
        vc = sb('vc', [128, 2, 2, 64], BF16)
        pT = [sb('pT%d' % i, [128, 512], BF16) for i in range(10)]
        zt = sb('zt', [64, 512])
        attT = [sb('attT%d' % i, [64, 8, 128], BF16) for i in range(2)]
        tok = [sb('tok%d' % i, [128, 512]) for i in range(2)]
        zsb = [sb('zsb%d' % i, [128, 512]) for i in range(2)]
        gt = sb('gt', [128, 16])
        gbs = sb('gbs', [128, 16])
        gl = sb('gl', [128, 16])
        gst = [sb('gst%d' % i, [128, 24]) for i in range(2)]
        w_in = self.w_in
        cnt = {'x': 0, 'pT': 0, 'cs': 0, 'tok': 0, 'att': 0}

        def project(kind, idx, si):
            own = kind == 'loc' and idx < NT
            need_q = kind == 'loc' and idx <= NT
            need_akv = kind == 'ctx' or (kind == 'loc' and idx <= NT)
            s = cnt['x'] % 2
            cnt['x'] += 1
            src = I['ctx'] if kind == 'ctx' else I['x']
            P.op('sp', DMA(xt[s][:], src[idx * 128:(idx + 1) * 128, :]), writes=['xt%d' % s], dma='xt%d' % s)
            for hh in range(2):
                P.op('dve', lambda e, hh=hh, s=s: e.bn_stats(out=st6[:, hh, :], in_=xt[s][:, hh * 512:(hh + 1) * 512]),
                     reads=['xt%d' % s], writes=[('st6', hh)])
            P.op('dve', lambda e: e.bn_aggr(out=mv[:], in_=st6[:].rearrange("p a b -> p (a b)")), reads=[('st6', 0), ('st6', 1)], writes=['mv'])
            P.op('act', ACT(rstd[:], mv[:, 1:2], AF.Ln, bias=self.epsc[:, 0:1]), reads=['mv', 'epsc0'], writes=['rstd'])
            P.op('act', ACT(rstd[:], rstd[:], AF.Exp, scale=-0.5), reads=['rstd'], writes=['rstd'])
            P.op('dve', TS(xn[:], xt[s][:], mv[:, 0:1], ALU.subtract, rstd[:, 0:1], ALU.mult),
                 reads=['xt%d' % s, 'mv', 'rstd'], writes=['xn'])
            if kind == 'ctx':
                sh, sc, mk = self.modc[:, 0:D], self.modc[:, D:2 * D], self.MODCK
            else:
                sh, sc, mk = self.modb[:, 0:D], self.modb[:, D:2 * D], self.MODK[0:4]
            P.op('pool', TT(xn[:], xn[:], sc, ALU.mult), reads=['xn'] + mk, writes=['xn'])
            P.op('dve', TT(hb[s][:], xn[:], sh, ALU.add), reads=['xn'] + mk, writes=['hb%d' % s])
            for kc in range(8):
                P.op('pe', TR(self.psb[:, kc * 128:(kc + 1) * 128], hb[s][:, kc * 128:(kc + 1) * 128], self.ident_b),
                     reads=['hb%d' % s, 'cb'], writes=['psb'])
            P.op('act', ACP(hT[s][:].rearrange("p a b -> p (a b)"), self.psb[:, :]), reads=['psb'], writes=['hT%d' % s])
            hk = 'hT%d' % s
            if self.stop_after == 'projA':
                return

            def fm_group(ps, pk, specs):
                for (o, c0, M) in specs:
                    for kc in range(8):
                        P.op('pe', MM(o, w_in[:, kc, c0:c0 + M], hT[s][:, kc, :], kc == 0, kc == 7),
                             reads=[hk] + self.WINK, writes=[pk])

            if kind == 'loc' and idx <= NT:
                r = idx % 2
                P.op('sp', DMA(rC[r][:], I['ropeC'][:, idx * 128:(idx + 1) * 128]), writes=['rC%d' % r], dma='rC%d' % r)
                P.op('sp', DMA(rS[r][:], I['ropeS'][:, idx * 128:(idx + 1) * 128]), writes=['rS%d' % r], dma='rS%d' % r)
                Cb = rC[r][:, :].unsqueeze(1).to_broadcast([64, 2, 128])
                Sb = rS[r][:, :].unsqueeze(1).to_broadcast([64, 2, 128])
                groups = []
                if own:
                    for p in range(4):
                        groups.append(('q', p))
                groups.append(('k', 0))
                for (typ, p) in groups:
                    ps, pk = self.psum()
                    psv = ps[0:64, :].rearrange("p (h x t) -> p h x t", h=2, x=2)
                    specs = []
                    for hh in range(2):
                        hd = 2 * p + hh
                        c_x = (QA if typ == 'q' else KA) + 64 * hd
                        c_p = (QAP if typ == 'q' else KAP) + 64 * hd
                        specs.append((psv[:, hh, 0, :], c_x, 64))
                        specs.append((psv[:, hh, 1, :], c_p, 64))
                    fm_group(ps, pk, specs)
                    P.op('dve', TT(rt1[:], psv[:, :, 0, :], Cb, ALU.mult), reads=[pk, 'rC%d' % r], writes=['rt1'])
                    P.op('dve', TT(rt2[:], psv[:, :, 1, :], Sb, ALU.mult), reads=[pk, 'rS%d' % r], writes=['rt2'])
                    if typ == 'q':
                        dst, dk_ = qTr[idx % 2][:, 2 * p:2 * p + 2, :], ('qTr', idx % 2, p)
                    else:
                        dst, dk_ = kTr[idx % 4][:, :, :], 'kTr%d' % (idx % 4)
                    P.op('pool', TT(dst, rt1[:], rt2[:], ALU.add), reads=['rt1', 'rt2'], writes=[dk_])
            elif kind == 'ctx':
                ps, pk = self.psum()
                psv = ps[0:64, 0:256].rearrange("p (h t) -> p h t", h=2)
                fm_group(ps, pk, [(psv[:, hh, :], KA + 64 * hh, 64) for hh in range(2)])
                P.op('act', ACP(kTc[:, idx, :, :], psv), reads=[pk], writes=[('kTc', idx)])
            if self.stop_after == 'projB':
                return
            ps_i = si % 3
            chunks = list(range(12)) if need_q else list(range(4, 12))
            for g0 in range(0, 12, 4):
                js = [j for j in chunks if g0 <= j < g0 + 4]
                if not js:
                    continue
                ps, pk = self.psum()
                psv = ps[:, :].rearrange("p (j t) -> p j t", j=4)
                fm_group(ps, pk, [(psv[:, j - g0, :], DQ + 128 * j, 128) for j in js])
                P.op('act', ACP(pad[ps_i][:, js[0]:js[-1] + 1, 2:130], psv[:, js[0] - g0:js[-1] - g0 + 1, :]),
                     reads=[pk], writes=[('pad', ps_i, g0)])
            if self.stop_after == 'projC':
                return
            if own:
                ps, pk = self.psum()
                for kc in range(8):
                    P.op('pe', MM(ps[:, :], hT[s][:, kc, :], w_in[:, kc, ZC:ZC + 512], kc == 0, kc == 7),
                         reads=[hk] + self.WINK, writes=[pk])
                zi = idx % 2
                P.op('act', ACT(zsb[zi][:], ps[:, :], AF.Silu), reads=[pk], writes=['zsb%d' % zi])
                P.op('pool', TT(zsb[zi][:], zsb[zi][:], self.wnbc[:].rearrange("p a b -> p (a b)"), ALU.mult),
                     reads=['zsb%d' % zi, 'wnbc'], writes=['zsb%d' % zi])
                P.op('sp', DMA(S['zs'][idx * 128:(idx + 1) * 128, :], zsb[zi][:]), reads=['zsb%d' % zi], dma='zsb%d' % zi)
            ps, pk = self.psum()
            for kc in range(8):
                P.op('pe', MM(ps[:, 0:144], hT[s][:, kc, :], w_in[:, kc, VA:VA + 144], kc == 0, kc == 7),
                     reads=[hk] + self.WINK, writes=[pk])
            if need_akv:
                if kind == 'ctx':
                    P.op('act', ACP(vc[:, idx, :, :], ps[:, 0:128].rearrange("p (h d) -> p h d", h=2)), reads=[pk], writes=[('vc', idx), 'vcopy'])
                else:
                    P.op('act', ACP(vr[idx % 4][:, :, :], ps[:, 0:128].rearrange("p (h d) -> p h d", h=2)), reads=[pk], writes=['vr%d' % (idx % 4), 'vcopy'])
            if self.stop_after == 'projD':
                return
            gi = cnt['x'] % 2
            g0tok = (idx * 128) if kind == 'ctx' else (256 + idx * 128)
            P.op('act', ACP(gbs[:], ps[:, 128:144]), reads=[pk, 'vcopy'], writes=['gbs'])
            P.op('dve', TS(gt[:, 0:8], gbs[:, 0:8], -1.0, ALU.mult), reads=['gbs'], writes=['gt0'])
            P.op('dve', TT(gt[:, 8:16], gbs[:, 8:16], self.dtb, ALU.add), reads=['gbs', 'adt'], writes=['gt1'])
            P.op('act', ACT(gl[:], gt[:], AF.Exp), reads=['gt0', 'gt1'], writes=['gl'])
            P.op('act', ACT(gl[:], gl[:], AF.Ln, bias=self.epsc[:, 2:3]), reads=['gl', 'epsc2'], writes=['gl'])
            P.op('dve', TT(gst[gi][:, 0:8], gl[:, 8:16], self.nexp[:], ALU.mult), reads=['gl', 'nexp'], writes=[('gst', gi, 0)])
            P.op('act', ACT(gst[gi][:, 8:16], gl[:, 0:8], AF.Exp, scale=-1.0), reads=['gl'], writes=[('gst', gi, 1)])
            P.op('dve', TS(gst[gi][:, 16:24], gl[:, 0:8], -1.0, ALU.mult), reads=['gl'], writes=[('gst', gi, 2)])
            if self.stop_after != 'projE':
              P.op('sp', DMA(S['gate'][g0tok:g0tok + 128, :], gst[gi][:]), reads=[('gst', gi, k) for k in range(3)],
                   writes=[('gate_s', g0tok)], dma='gst%d' % gi)

        def post(kind, idx, si):
            own = kind == 'loc' and idx < NT
            ps_i = si % 3
            g0tok = (idx * 128) if kind == 'ctx' else (256 + idx * 128)
            c = cnt['cs'] % 2
            cnt['cs'] += 1
            chunks = list(range(12)) if own else list(range(4, 12))
            padk = [('pad', ps_i, g0) for g0 in (0, 4, 8)] + [('padL', ps_i), ('padR', ps_i)]
            for j in chunks:
                P.op('dve', TS(cs[c][:, j, :], pad[ps_i][:, j, 0:128], self.cw[:, j, 0:1], ALU.mult),
                     reads=padk + ['cw'], writes=[('cs', c, j)])
                for k in range(1, 5):
                    P.op('dve', STT(cs[c][:, j, :], pad[ps_i][:, j, k:k + 128], self.cw[:, j, k:k + 1], cs[c][:, j, :], ALU.mult, ALU.add),
                         reads=padk + ['cw', ('cs', c, j)], writes=[('cs', c, j)])
            j0, j1 = chunks[0], chunks[-1] + 1
            csk = [('cs', c, j) for j in chunks]
            P.op('act', ACT(cs[c][:, j0:j1, :], cs[c][:, j0:j1, :], AF.Silu), reads=csk, writes=csk)
            qk0 = 0 if own else 4
            nq = 8 - qk0
            P.op('pool', TT(sq[:, qk0:8, :], cs[c][:, qk0:8, :], cs[c][:, qk0:8, :], ALU.mult), reads=csk, writes=['sq'])
            for g0 in range(qk0, 8, 4):
                ps, pk = self.psum()
                for j in range(g0, g0 + 4):
                    P.op('pe', MM(ps[:, (j - g0) * 128:(j - g0 + 1) * 128], self.ones_f, sq[:, j, :], True, True),
                         reads=['cf', 'sq'], writes=[pk])
                if g0 == 0:
                    P.op('act', ACT(rn[:, 0:4, :].rearrange("p a b -> p (a b)"), ps[:, :], AF.Ln, scale=128.0, bias=self.epsc[:, 1:2]),
                         reads=[pk, 'epsc1'], writes=[('rn', 0)])
                else:
                    P.op('act', ACT(rn[:, 4:8, :].rearrange("p a b -> p (a b)"), ps[:, :], AF.Ln, bias=self.epsc[:, 0:1]),
                         reads=[pk, 'epsc0'], writes=[('rn', 4)])
                P.op('act', ACT(rn[:, g0:g0 + 4, :], rn[:, g0:g0 + 4, :], AF.Exp, scale=-0.5), reads=[('rn', g0)], writes=[('rn', g0)])
                P.op('dve', TT(cs[c][:, g0:g0 + 4, :], cs[c][:, g0:g0 + 4, :], rn[:, g0:g0 + 4, :], ALU.mult),
                     reads=[('rn', g0)] + csk, writes=[('cs', c, j) for j in range(g0, g0 + 4)])
            P.op('sp', DMA(S['kT'][:, :, g0tok:g0tok + 128].rearrange("h d t -> d h t"), cs[c][:, 4:8, :]),
                 reads=[('cs', c, j) for j in range(4, 8)], writes=[('kT_s', g0tok)], dma='cs%d' % c)
            if own:
                P.op('sp', DMA(S['qT'][:, :, idx * 128:(idx + 1) * 128].rearrange("h d t -> d h t"), cs[c][:, 0:4, :]),
                     reads=[('cs', c, j) for j in range(0, 4)], writes=[('qT_s', idx)], dma='cs%d' % c)
            for (which, j0_, dst) in (('k', 4, S['ktok']), ('v', 8, S['vtok'])):
                ps, pk = self.psum()
                for j in range(4):
                    P.op('pe', TR(ps[:, j * 128:(j + 1) * 128], cs[c][:, j0_ + j, :], self.ident_f),
                         reads=[('cs', c, j0_ + j), 'cf'], writes=[pk])
                ti = cnt['tok'] % 2
                cnt['tok'] += 1
                P.op('act', ACP(tok[ti][:], ps[:, :]), reads=[pk], writes=['tok%d' % ti])
                P.op('sp', DMA(dst[g0tok:g0tok + 128, :], tok[ti][:]), reads=['tok%d' % ti], writes=[(which + 'tok_s', g0tok)], dma='tok%d' % ti)

        def attention(qb):
            qs = qTr[qb % 2]
            blocks = []
            if qb > 0:
                blocks.append(('L', kTr[(qb - 1) % 4], vr[(qb - 1) % 4], 'kTr%d' % ((qb - 1) % 4), 'vr%d' % ((qb - 1) % 4)))
            blocks.append(('C', kTr[qb % 4], vr[qb % 4], 'kTr%d' % (qb % 4), 'vr%d' % (qb % 4)))
            blocks.append(('R', kTr[(qb + 1) % 4], vr[(qb + 1) % 4], 'kTr%d' % ((qb + 1) % 4), 'vr%d' % ((qb + 1) % 4)))
            blocks.append(('X0', None, None, ('kTc', 0), ('vc', 0)))
            blocks.append(('X1', None, None, ('kTc', 1), ('vc', 1)))
            ai = cnt['att'] % 2
            cnt['att'] += 1
            qk = [('qTr', qb % 2, p) for p in range(4)]
            for kvh in range(2):
                pts = []
                for (typ, kt, vt, kk, vk) in blocks:
                    ps, pk = self.psum()
                    if typ[0] == 'X':
                        lhs = kTc[:, int(typ[1]), kvh, :]
                    else:
                        lhs = kt[:, kvh, :]
                    P.op('pe', MM(ps[:, :], lhs, qs[:, 4 * kvh:4 * kvh + 4, :].rearrange("p g q -> p (g q)"), True, True),
                         reads=[kk] + qk, writes=[pk])
                    pi = cnt['pT'] % 10
                    cnt['pT'] += 1
                    P.op('act', ACT(pT[pi][:], ps[:, :], AF.Exp, scale=0.125), reads=[pk], writes=['pT%d' % pi])
                    if typ in ('L', 'R'):
                        m = self.maskL if typ == 'L' else self.maskR
                        P.op('pool', TT(pT[pi][:].rearrange("p (g q) -> p g q", g=4), pT[pi][:].rearrange("p (g q) -> p g q", g=4),
                                        m.unsqueeze(1).to_broadcast([128, 4, 128]), ALU.mult),
                             reads=['pT%d' % pi, 'cb'], writes=['pT%d' % pi])
                    pts.append((typ, pi, vt, vk))
                pso, pko = self.psum()
                psz, pkz = self.psum()
                for bi, (typ, pi, vt, vk) in enumerate(pts):
                    if typ[0] == 'X':
                        lhs = vc[:, int(typ[1]), kvh, :]
                    else:
                        lhs = vt[:, kvh, :]
                    P.op('pe', MM(pso[0:64, :], lhs, pT[pi][:], bi == 0, bi == len(pts) - 1), reads=[vk, 'pT%d' % pi], writes=[pko])
                for bi, (typ, pi, vt, vk) in enumerate(pts):
                    P.op('pe', MM(psz[0:64, :], self.ones_b[:, 0:64], pT[pi][:], bi == 0, bi == len(pts) - 1), reads=['cb', 'pT%d' % pi], writes=[pkz])
                P.op('dve', TT(zt[:], psz[0:64, :], self.esink[:, 4 * kvh:4 * kvh + 4, :].rearrange("p g q -> p (g q)"), ALU.add),
                     reads=[pkz, 'esink'], writes=['zt'])
                P.op('dve', RECIP(zt[:], zt[:]), reads=['zt'], writes=['zt'])
                P.op('dve', TT(attT[ai][:, 4 * kvh:4 * kvh + 4, :].rearrange("p g q -> p (g q)"), pso[0:64, :], zt[:], ALU.mult),
                     reads=[pko, 'zt'], writes=[('attT', ai, kvh)])
            P.op('sp', DMA(S['attnT'][:, :, qb * 128:(qb + 1) * 128], attT[ai][:]), reads=[('attT', ai, 0), ('attT', ai, 1)],
                 writes=[('attnT_s', qb)], dma='attT%d' % ai)

        def run_seq(kind, tiles):
            for si, idx in enumerate(tiles):
                project(kind, idx, si)
                p = si % 3
                if si == 0:
                    P.op('pool', MEMSET(pad[p][:, :, 0:2], 0.0), writes=[('padL', p)])
                else:
                    pp = (si - 1) % 3
                    pk_prev = [('pad', pp, g0) for g0 in (0, 4, 8)]
                    pk_cur = [('pad', p, g0) for g0 in (0, 4, 8)]
                    P.op('pool', CP(pad[p][:, :, 0:2], pad[pp][:, :, 128:130]), reads=pk_prev, writes=[('padL', p)])
                    P.op('pool', CP(pad[pp][:, :, 130:132], pad[p][:, :, 2:4]), reads=pk_cur, writes=[('padR', pp)])
                    if self.stop_after not in ('proj', 'projA', 'projB', 'projC', 'projD', 'projE'):
                        post(kind, tiles[si - 1], si - 1)
                    if kind == 'loc' and 1 <= idx <= NT and self.stop_after not in ('proj', 'post', 'projA', 'projB', 'projC', 'projD', 'projE'):
                        attention(idx - 1)
            p = (len(tiles) - 1) % 3
            P.op('pool', MEMSET(pad[p][:, :, 130:132], 0.0), writes=[('padR', p)])
            if self.stop_after not in ('proj', 'projA', 'projB', 'projC', 'projD', 'projE'):
                post(kind, tiles[-1], len(tiles) - 1)

        for p in range(3):
            P.op('pool', MEMSET(pad[p][:], 0.0), writes=[('pad', p, g0) for g0 in (0, 4, 8)] + [('padL', p), ('padR', p)])
        run_seq('ctx', [0, 1])
        run_seq('loc', list(range(NL)))
        P.barrier()
        es.close()
        self.esA.close()


    def t_steps(self, es):
        P, I, S = self.P, self.I, self.S
        sb = lambda name, shape, dt=F32: self.sb('t_' + name, shape, dt, es=es)
        tu = [sb('tu%d' % i, [128, 2, D]) for i in range(2)]
        tv = [sb('tv%d' % i, [128, 2, D]) for i in range(2)]
        to = [sb('to%d' % i, [128, 2, 2 * D], BF16) for i in range(2)]
        return self._t_gen(tu, tv, to)

    def _t_gen(self, tu, tv, to):
        P, I, S = self.P, self.I, self.S
        for st in range(64):
            b = st % 2
            r0 = st * 256
            P.op('sp', DMA(tu[b][:], I['peer_u'][r0:r0 + 256, :].rearrange("(p j) d -> p j d", j=2)), writes=['tu%d' % b], dma='t_u%d' % b)
            P.op('act', DMA(tv[b][:], I['peer_v'][r0:r0 + 256, :].rearrange("(p j) d -> p j d", j=2)), writes=['tv%d' % b], dma='t_v%d' % b)
            P.op('act', ACP(to[b][:, :, 0:D], tu[b][:]), reads=['tu%d' % b], writes=[('to', b, 0)])
            P.op('dve', CP(to[b][:, :, D:2 * D], tv[b][:]), reads=['tv%d' % b], writes=[('to', b, 1)])
            P.op('sp', DMA(S['uv'][r0:r0 + 256, :].rearrange("(p j) d -> p j d", j=2), to[b][:]), reads=[('to', b, 0), ('to', b, 1)], dma='t_o%d' % b)
            yield

    def layer_norm_tile(self, pre, src, srck, dst, dstk, st6, mv, rstd):
        P = self.P
        for hh in range(2):
            P.op('dve', (lambda hh: lambda e: e.bn_stats(out=st6[:, hh, :], in_=src[:, hh * 512:(hh + 1) * 512]))(hh),
                 reads=srck, writes=[(pre + 'st6', hh)])
        P.op('dve', lambda e: e.bn_aggr(out=mv[:], in_=st6[:].rearrange("p a b -> p (a b)")), reads=[(pre + 'st6', 0), (pre + 'st6', 1)], writes=[pre + 'mv'])
        P.op('act', ACT(rstd[:], mv[:, 1:2], AF.Ln, bias=self.epsc[:, 0:1]), reads=[pre + 'mv'], writes=[pre + 'rstd'])
        P.op('act', ACT(rstd[:], rstd[:], AF.Exp, scale=-0.5), reads=[pre + 'rstd'], writes=[pre + 'rstd'])
        P.op('dve', TS(dst[:], src[:], mv[:, 0:1], ALU.subtract, rstd[:, 0:1], ALU.mult), reads=srck + [pre + 'mv', pre + 'rstd'], writes=dstk)

    def phaseM(self):
        P, I, S, NT = self.P, self.I, self.S, self.NT
        es = ExitStack()
        sb = lambda name, shape, dt=F32: self.sb('m_' + name, shape, dt, es=es)
        self.lnbc = sb('lnbc', [128, 4, D])
        P.op('sp', DMA(self.lnbc[:].rearrange("p a b -> p (a b)"), I['lnp'][:, :]), writes=['lnbc'], dma='*')
        wo_a = sb('wo_a', [64, 8, D], BF16)
        wo_g = sb('wo_g', [128, 4, D], BF16)
        for h in range(8):
            P.op('pool', DMA(wo_a[:, h, :], I['w_out'][h * 64:(h + 1) * 64, :]), writes=[('wo_a', h)], dma='m_wo')
        for c in range(4):
            P.op('pool', DMA(wo_g[:, c, :], I['w_out'][512 + c * 128:512 + (c + 1) * 128, :]), writes=[('wo_g', c)], dma='m_wo')
        WOK = [('wo_a', h) for h in range(8)] + [('wo_g', c) for c in range(4)]
        oa = [sb('oa%d' % i, [128, 4, 128]) for i in range(2)]
        obt = [sb('ob%d' % i, [128, 4, 128]) for i in range(2)]
        zs = [sb('zs%d' % i, [128, 512]) for i in range(2)]
        at = [sb('at%d' % i, [64, 8, 128], BF16) for i in range(2)]
        xt = [sb('xt%d' % i, [128, D]) for i in range(2)]
        osq = sb('osq', [128, 4, 128])
        ms = sb('ms', [128, 4])
        gdn = sb('gdn', [128, 512], BF16)
        gT = sb('gT', [128, 4, 128], BF16)
        rr = sb('rr', [128, D])
        st6 = sb('st6', [128, 2, 6]); mv = sb('mv', [128, 2]); rstd = sb('rstd', [128, 1])
        x1 = [sb('x1%d' % i, [128, D]) for i in range(2)]
        f2 = lambda t: t[:].rearrange("p a b -> p (a b)")
        for idx in range(NT):
            b = idx % 2
            r0 = idx * 128
            P.op('sp', DMA(f2(oa[b]), S['oA'][r0:r0 + 128, :]), reads=[('ga_o_s', idx)], writes=['oa%d' % b], dma='m_oa%d' % b)
            P.op('sp', DMA(f2(obt[b]), S['oB'][r0:r0 + 128, :]), reads=[('gb_o_s', idx)], writes=['ob%d' % b], dma='m_ob%d' % b)
            P.op('sp', DMA(zs[b][:], S['zs'][r0:r0 + 128, :]), writes=['zs%d' % b], dma='m_zs%d' % b)
            P.op('sp', DMA(at[b][:], S['attnT'][:, :, r0:r0 + 128]), writes=['at%d' % b], dma='m_at%d' % b)
            P.op('sp', DMA(xt[b][:], I['x'][r0:r0 + 128, :]), writes=['xt%d' % b], dma='m_xt%d' % b)
            P.op('pool', TT(f2(oa[b]), f2(oa[b]), f2(obt[b]), ALU.add), reads=['oa%d' % b, 'ob%d' % b], writes=['oa%d' % b])
            P.op('pool', TT(f2(osq), f2(oa[b]), f2(oa[b]), ALU.mult), reads=['oa%d' % b], writes=['osq'])
            P.op('dve', lambda e, b=b: e.tensor_reduce(out=ms[:], in_=osq[:], axis=AX.X, op=ALU.add), reads=['osq'], writes=['ms'])
            P.op('act', ACT(ms[:], ms[:], AF.Ln, scale=1.0 / 128.0, bias=self.epsc[:, 0:1]), reads=['ms'], writes=['ms'])
            P.op('act', ACT(ms[:], ms[:], AF.Exp, scale=-0.5), reads=['ms'], writes=['ms'])
            P.op('dve', TT(oa[b][:], oa[b][:], ms[:, :].unsqueeze(2).to_broadcast([128, 4, 128]), ALU.mult), reads=['oa%d' % b, 'ms'], writes=['oa%d' % b])
            P.op('pool', TT(gdn[:], f2(oa[b]), zs[b][:], ALU.mult), reads=['oa%d' % b, 'zs%d' % b], writes=['gdn'])
            for c in range(4):
                P.op('pe', TR(self.psb[:, c * 128:(c + 1) * 128], gdn[:, c * 128:(c + 1) * 128], self.ident_b), reads=['gdn', 'cb'], writes=['psb'])
            P.op('act', ACP(f2(gT), self.psb[:, 0:512]), reads=['psb'], writes=['gT'])
            for nh in range(2):
                ps, pk = self.psum()
                ns = slice(nh * 512, (nh + 1) * 512)
                for h in range(8):
                    P.op('pe', MM(ps[:, :], at[b][:, h, :], wo_a[:, h, ns], h == 0, False), reads=['at%d' % b] + WOK, writes=[pk])
                for c in range(4):
                    P.op('pe', MM(ps[:, :], gT[:, c, :], wo_g[:, c, ns], False, c == 3), reads=['gT'] + WOK, writes=[pk])
                P.op('dve', TT(rr[:, ns], ps[:, :], self.modb[:, 2 * D + nh * 512:2 * D + (nh + 1) * 512], ALU.mult), reads=[pk], writes=[('rr', nh)])
            P.op('dve', STT(rr[:], xt[b][:], ALPHA, rr[:], ALU.mult, ALU.add), reads=['xt%d' % b, ('rr', 0), ('rr', 1)], writes=[('rr', 0), ('rr', 1)])
            self.layer_norm_tile('m_', rr, [('rr', 0), ('rr', 1)], rr, [('rr', 0), ('rr', 1)], st6, mv, rstd)
            P.op('pool', TT(rr[:], rr[:], self.lnbc[:, 0, :], ALU.mult), reads=[('rr', 0), ('rr', 1), 'lnbc'], writes=[('rr', 0), ('rr', 1)])
            P.op('dve', TT(x1[b][:], rr[:], self.lnbc[:, 1, :], ALU.add), reads=[('rr', 0), ('rr', 1), 'lnbc'], writes=['x1%d' % b])
            P.op('pool', DMA(S['x1'][r0:r0 + 128, :], x1[b][:]), reads=['x1%d' % b], writes=[('x1_s', idx)], dma='m_x1%d' % b)
        P.barrier()
        es.close()

    def phaseC(self):
        P, I, S, NT = self.P, self.I, self.S, self.NT
        es = ExitStack()
        sb = lambda name, shape, dt=F32: self.sb('c_' + name, shape, dt, es=es)
        self.lnbc = sb('lnbc', [128, 4, D])
        P.op('sp', DMA(self.lnbc[:].rearrange("p a b -> p (a b)"), I['lnp'][:, :]), writes=['lnbc'], dma='*')
        wq = sb('wq', [128, 8, 2048], BF16)
        for kc in range(8):
            P.op('pool', DMA(wq[:, kc, :], I['wq'][kc * 128:(kc + 1) * 128, :]), writes=[('wq', kc)], dma='c_wq')
        WQK = [('wq', kc) for kc in range(8)]
        skT = sb('skT', [128, 2, 128])
        P.op('sp', DMA(skT[:], I['skT'][:, :, :]), writes=['skT'], dma='*')
        iota16 = self.cf[:, 832:848]
        x1t = [sb('x1t%d' % i, [128, D]) for i in range(2)]
        xn = sb('xn', [128, D])
        h2_2 = [sb('h2_%d' % i, [128, D]) for i in range(2)]
        h2b = sb('h2b', [128, D], BF16)
        h2T = sb('h2T', [128, 8, 128], BF16)
        st6 = sb('st6', [128, 2, 6]); mv = sb('mv', [128, 2]); rstd = sb('rstd', [128, 1])
        st6b = sb('st6b', [128, 2, 6]); mvb = sb('mvb', [128, 2]); rstdb = sb('rstdb', [128, 1])
        sc = sb('sc', [128, 16, 128]); sc2 = sb('sc2', [128, 16, 128])
        qTs = sc2
        stop_ = sb('stop', [128, 16, 16]); itop = sb('itop', [128, 16, 16], U32); itf = sb('itf', [128, 16, 16])
        cand = sb('cand', [128, 8, 256])
        cand2 = sc[:].rearrange("p a b -> p (a b)").rearrange("p (h c) -> p h c", h=8)
        best = sb('best', [128, 8, 16]); pos = sb('pos', [128, 8, 16], U32)
        pa = sb('pa', [128, 8, 16], I32); pb = sb('pb', [128, 8, 16], I32)
        paf = sb('paf', [128, 8, 16]); pbf = sb('pbf', [128, 8, 16])
        eq = sb('eq', [128, 8, 16, 16], BF16)
        i1 = sb('i1', [128, 8, 16]); i2 = sb('i2', [128, 8, 16])
        eidf = sb('eidf', [128, 128]); eidx_2 = [sb('eidx%d' % i, [128, 128], I32) for i in range(2)]
        ge_2 = [sb('ge%d' % i, [128, 8, 16]) for i in range(2)]; gsum = sb('gsum', [128, 8])
        apre = sb('apre', [128, 128]); wgt = sb('wgt', [128, 128])
        junk = sb('junk', [128, D], BF16)
        yacc = sb('yacc', [128, D])
        ot = [sb('ot0', [128, D])] * 2
        wg4 = sb('wg4', [128, 128])
        dg = [sb('dg%d' % i, [128, 128], BF16) for i in range(8)]
        NR = int(min(16, self.nc.sbuf_bytes_remaining // 4096 - 1))
        assert NR >= 8, NR
        ring = [sb('ring%d' % i, [128, 2 * D], BF16) for i in range(NR)]
        f2 = lambda t: t[:].rearrange("p a b -> p (a b)")
        rcnt = 0
        self.ps_lim = 5
        self.ps_next = 0
        def front_gen(idx):
            b = idx % 2
            r0 = idx * 128
            h2, eidx, ge = h2_2[b], eidx_2[b], ge_2[b]
            kh2, keidx, kge = 'h2_%d' % b, 'eidx%d' % b, 'ge%d' % b
            P.op('sp', DMA(x1t[b][:], S['x1'][r0:r0 + 128, :]), reads=[('x1_s', idx)], writes=['x1t%d' % b], dma='c_x1t%d' % b)
            self.layer_norm_tile('c_', x1t[b], ['x1t%d' % b], xn, ['xn'], st6, mv, rstd)
            P.op('pool', TT(xn[:], xn[:], self.modb[:, 4 * D:5 * D], ALU.mult), reads=['xn'], writes=['xn'])
            P.op('dve', TT(h2[:], xn[:], self.modb[:, 3 * D:4 * D], ALU.add), reads=['xn'], writes=[kh2])
            P.op('act', ACP(h2b[:], h2[:]), reads=[kh2], writes=['h2b'])
            yield
            for kc in range(8):
                P.op('pe', TR(self.psb[:, kc * 128:(kc + 1) * 128], h2b[:, kc * 128:(kc + 1) * 128], self.ident_b), reads=['h2b', 'cb'], writes=['psb'])
            P.op('act', ACP(f2(h2T), self.psb[:, :]), reads=['psb'], writes=['h2T'])
            yield
            for g0 in range(0, 16, 4):
                ps, pk = self.psum()
                for hx in range(g0, g0 + 4):
                    for kc in range(8):
                        P.op('pe', MM(ps[:, (hx - g0) * 128:(hx - g0 + 1) * 128], wq[:, kc, hx * 128:(hx + 1) * 128], h2T[:, kc, :], kc == 0, kc == 7),
                             reads=['h2T'] + WQK, writes=[pk])
                P.op('act', ACP(qTs[:, g0:g0 + 4, :].rearrange("p a b -> p (a b)"), ps[:, :]), reads=[pk], writes=[('qTs', g0), 'sc2all'])
                yield
            for g0 in range(0, 16, 4):
                ps, pk = self.psum()
                for hx in range(g0, g0 + 4):
                    P.op('pe', MM(ps[:, (hx - g0) * 128:(hx - g0 + 1) * 128], qTs[:, hx, :], skT[:, hx % 2, :], True, True),
                         reads=[('qTs', g0), 'skT', 'sc2all'], writes=[pk])
                P.op('act', ACP(sc[:, g0:g0 + 4, :].rearrange("p a b -> p (a b)"), ps[:, :]), reads=[pk], writes=[('sc', g0), 'scall'])
                yield
            yield
            for hx in range(16):
                sk_ = ('sc', 4 * (hx // 4))
                P.op('dve', (lambda hx: lambda e: e.max(out=stop_[:, hx, 0:8], in_=sc[:, hx, :]))(hx), reads=[sk_, 'scall'], writes=[('stop', hx, 0)])
                P.op('dve', (lambda hx: lambda e: e.match_replace(out=sc2[:, hx, :], in_to_replace=stop_[:, hx, 0:8], in_values=sc[:, hx, :], imm_value=-1e30))(hx),
                     reads=[sk_, 'scall', ('stop', hx, 0)], writes=[('sc2', hx), 'sc2all'])
                P.op('dve', (lambda hx: lambda e: e.max(out=stop_[:, hx, 8:16], in_=sc2[:, hx, :]))(hx), reads=[('sc2', hx), 'sc2all'], writes=[('stop', hx, 1)])
                P.op('dve', (lambda hx: lambda e: e.max_index(out=itop[:, hx, 0:8], in_max=stop_[:, hx, 0:8], in_values=sc[:, hx, :]))(hx),
                     reads=[sk_, 'scall', ('stop', hx, 0)], writes=[('itop', hx, 0)])
                P.op('dve', (lambda hx: lambda e: e.max_index(out=itop[:, hx, 8:16], in_max=stop_[:, hx, 8:16], in_values=sc2[:, hx, :]))(hx),
                     reads=[('sc2', hx), 'sc2all', ('stop', hx, 1)], writes=[('itop', hx, 1)])
                if hx % 2 == 1:
                    yield
            yield
            STK = [('stop', hx, k) for hx in range(16) for k in range(2)]
            ITK = [('itop', hx, k) for hx in range(16) for k in range(2)]
            P.op('dve', CP(f2(itf), f2(itop)), reads=ITK, writes=['itf'])
            sv = stop_[:].rearrange("p (h x) a -> p h x a", x=2)
            iv = itf[:].rearrange("p (h x) a -> p h x a", x=2)
            c4 = cand[:].rearrange("p h (a b) -> p h a b", a=16)
            P.op('dve', TT(c4, sv[:, :, 0, :].unsqueeze(3).to_broadcast([128, 8, 16, 16]), sv[:, :, 1, :].unsqueeze(2).to_broadcast([128, 8, 16, 16]), ALU.add),
                 reads=STK, writes=['cand'])
            for h in range(8):
                P.op('dve', (lambda h: lambda e: e.max(out=best[:, h, 0:8], in_=cand[:, h, :]))(h), reads=['cand'], writes=[('best', h, 0)])
                P.op('dve', (lambda h: lambda e: e.match_replace(out=cand2[:, h, :], in_to_replace=best[:, h, 0:8], in_values=cand[:, h, :], imm_value=-1e30))(h),
                     reads=['cand', ('best', h, 0)], writes=[('cand2', h), 'scall'])
                P.op('dve', (lambda h: lambda e: e.max(out=best[:, h, 8:16], in_=cand2[:, h, :]))(h), reads=[('cand2', h), 'scall'], writes=[('best', h, 1)])
                P.op('dve', (lambda h: lambda e: e.max_index(out=pos[:, h, 0:8], in_max=best[:, h, 0:8], in_values=cand[:, h, :]))(h),
                     reads=['cand', ('best', h, 0)], writes=[('pos', h, 0)])
                P.op('dve', (lambda h: lambda e: e.max_index(out=pos[:, h, 8:16], in_max=best[:, h, 8:16], in_values=cand2[:, h, :]))(h),
                     reads=[('cand2', h), 'scall', ('best', h, 1)], writes=[('pos', h, 1)])
                if h % 2 == 1:
                    yield
            yield
            BK = [('best', h, k) for h in range(8) for k in range(2)]
            PK = [('pos', h, k) for h in range(8) for k in range(2)]
            posi = pos[:].bitcast(I32)
            P.op('dve', lambda e: e.tensor_single_scalar(out=f2(pa), in_=posi.rearrange("p a b -> p (a b)"), scalar=4, op=ALU.arith_shift_right), reads=PK, writes=['pa'])
            P.op('dve', lambda e: e.tensor_single_scalar(out=f2(pb), in_=posi.rearrange("p a b -> p (a b)"), scalar=15, op=ALU.bitwise_and), reads=PK, writes=['pb'])
            P.op('dve', CP(f2(paf), f2(pa)), reads=['pa'], writes=['paf'])
            P.op('dve', CP(f2(pbf), f2(pb)), reads=['pb'], writes=['pbf'])
            yield
            io4 = iota16.unsqueeze(1).unsqueeze(1).to_broadcast([128, 8, 16, 16])
            for (pf, pfk, x_, dst, dk_) in ((paf, 'paf', 0, i1, 'i1'), (pbf, 'pbf', 1, i2, 'i2')):
                P.op('dve', TT(eq[:], io4, pf[:, :, :].unsqueeze(3).to_broadcast([128, 8, 16, 16]), ALU.is_equal), reads=['cf', pfk], writes=['eq'])
                P.op('pool', TT(eq[:], eq[:], iv[:, :, x_, :].unsqueeze(2).to_broadcast([128, 8, 16, 16]), ALU.mult), reads=['eq', 'itf'], writes=['eq'])
                P.op('dve', (lambda dst: lambda e: e.tensor_reduce(out=dst[:], in_=eq[:], axis=AX.X, op=ALU.add))(dst), reads=['eq'], writes=[dk_])
            P.op('dve', STT(eidf[:], f2(i1), 128.0, f2(i2), ALU.mult, ALU.add), reads=['i1', 'i2'], writes=['eidf'])
            P.op('dve', CP(eidx[:], eidf[:]), reads=['eidf'], writes=[keidx])
            yield
            P.op('dve', TT(ge[:], best[:], best[:, :, 0:1].to_broadcast([128, 8, 16]), ALU.subtract), reads=BK, writes=[kge])
            P.op('act', ACT(f2(ge), f2(ge), AF.Exp), reads=[kge], writes=[kge])
            P.op('dve', lambda e: e.tensor_reduce(out=gsum[:], in_=ge[:], axis=AX.X, op=ALU.add), reads=[kge], writes=['gsum'])
            P.op('dve', RECIP(gsum[:], gsum[:]), reads=['gsum'], writes=['gsum'])
            P.op('dve', TT(ge[:], ge[:], gsum[:, :].unsqueeze(2).to_broadcast([128, 8, 16]), ALU.mult), reads=[kge, 'gsum'], writes=[kge])
            yield

        def loop_gen(idx):
            nonlocal rcnt
            b = idx % 2
            r0 = idx * 128
            h2, eidx, ge = h2_2[b], eidx_2[b], ge_2[b]
            kh2, keidx, kge = 'h2_%d' % b, 'eidx%d' % b, 'ge%d' % b
            psY = [(self.psf[5], 'ps5'), (self.psf[6], 'ps6')]
            GS = 4
            dcnt = 0
            for g in range(0, 128, GS):
                ris = []
                for slot in range(g, g + GS):
                    ri = rcnt % NR
                    rcnt += 1
                    ris.append(ri)
                    P.op('pool', (lambda ri, slot: lambda e: e.indirect_dma_start(out=ring[ri][:], out_offset=None, in_=S['uv'][:, :],
                         in_offset=bass.IndirectOffsetOnAxis(ap=eidx[:, slot:slot + 1], axis=0)))(ri, slot), reads=[keidx], writes=['ring%d' % ri], dma='c_ring%d' % ri)
                    P.op('dve', STT(junk[:], ring[ri][:, 0:D], 1.0, h2[:], ALU.mult, ALU.mult, accum=apre[:, slot:slot + 1]), reads=['ring%d' % ri, kh2], writes=[('apre', slot), 'junk'])
                AKg = [('apre', sl) for sl in range(g, g + GS)]
                P.op('act', ACT(wg4[:, g:g + GS], apre[:, g:g + GS], AF.Gelu), reads=AKg, writes=[('wg4', g)])
                P.op('dve', TT(wgt[:, g:g + GS], wg4[:, g:g + GS], f2(ge)[:, g:g + GS], ALU.mult), reads=[('wg4', g), kge], writes=[('wgt', g)])
                for k, slot in enumerate(range(g, g + GS)):
                    di = dcnt % 8
                    dcnt += 1
                    ri = ris[k]
                    P.op('act', ACT(dg[di][:], self.ident_b, AF.Copy, scale=wgt[:, slot:slot + 1]), reads=['cb', ('wgt', g)], writes=['dg%d' % di])
                    for nh in range(2):
                        P.op('pe', MM(psY[nh][0][:, :], dg[di][:], ring[ri][:, D + nh * 512:D + (nh + 1) * 512], slot == 0, slot == 127),
                             reads=['dg%d' % di, 'ring%d' % ri], writes=[psY[nh][1]])
                yield
            yield
            for nh in range(2):
                P.op('dve', TT(yacc[:, nh * 512:(nh + 1) * 512], psY[nh][0][:, :], self.modb[:, 5 * D + nh * 512:5 * D + (nh + 1) * 512], ALU.mult),
                     reads=[psY[nh][1]], writes=[('yacc', nh)])
            P.op('dve', STT(yacc[:], x1t[b][:], ALPHA, yacc[:], ALU.mult, ALU.add), reads=['x1t%d' % b, ('yacc', 0), ('yacc', 1)], writes=['yacc', ('yacc', 0), ('yacc', 1)])
            self.layer_norm_tile('c2_', yacc, ['yacc'], yacc, ['yacc'], st6b, mvb, rstdb)
            P.op('pool', TT(yacc[:], yacc[:], self.lnbc[:, 2, :], ALU.mult), reads=['yacc', 'lnbc'], writes=['yacc'])
            P.op('dve', TT(ot[b][:], yacc[:], self.lnbc[:, 3, :], ALU.add), reads=['yacc', 'lnbc'], writes=['ot0'])
            P.op('sp', DMA(self.out[r0:r0 + 128, :], ot[b][:]), reads=['ot0'], dma='c_ot%d' % b)
            yield

        def drain(*gens):
            gens = [x for x in gens if x is not None]
            while gens:
                for x in list(gens):
                    try:
                        next(x)
                    except StopIteration:
                        gens.remove(x)
        prev = None
        for idx in range(NT):
            drain(front_gen(idx), prev)
            prev = loop_gen(idx)
        drain(prev)
        self.ps_lim = 7
        P.barrier()
        es.close()

    def gdn_pass(self, tag, base, go, tiles, out_ap, extra=None):
        P, S = self.P, self.S
        es = ExitStack()
        sb = lambda name, shape, dt=F32: self.sb(tag + name, shape, dt, es=es)
        cf = self.cf
        U = cf[0:64, base:base + 64]
        mbiT = cf[0:64, base + 64:base + 128]
        mbsT = cf[0:64, base + 128:base + 192]
        mbs = cf[0:64, base + 192:base + 256]
        I64 = cf[0:64, 768:832]
        ones64 = cf[0:64, 128:192]
        ones64x128 = cf[0:64, 128:256]
        kTt = [sb('kTt%d' % i, [128, 4, 128]) for i in range(2)]
        qTt = [sb('qTt%d' % i, [128, 4, 128]) for i in range(2)]
        ktk = [sb('ktk%d' % i, [64, 2, 4, 128]) for i in range(2)]
        vtk = [sb('vtk%d' % i, [64, 2, 4, 128]) for i in range(2)]
        gts = [sb('gts%d' % i, [64, 2, 24]) for i in range(2)]
        gsb = sb('gsb', [128, 16])
        egam = sb('egam', [64, 8])
        ekd = sb('ekd', [64, 8])
        egt2 = [sb('egt%d' % i, [128, 8]) for i in range(2)]
        bege = sb('bege', [64, 8])
        gpl = sb('gpl', [64, 8])
        G1 = sb('G1', [64, 8, 64]); G1n = sb('G1n', [64, 8, 64]); G1b = sb('G1b', [64, 8, 64])
        MaT = sb('MaT', [64, 8, 64]); Ma = sb('Ma', [64, 8, 64]); Dq = sb('Dq', [64, 8, 64])
        egb = sb('egb', [128, 8, 64])
        DTX = F32
        XY = [[sb('X%d' % i, [64, 8, 64], DTX), sb('Y%d' % i, [64, 8, 64], DTX)] for i in range(2)]
        R = sb('R', [64, 8, 64], DTX)
        QKm2 = [sb('QKm%d' % i, [64, 8, 64], BF16) for i in range(2)]
        vb = sb('vb', [64, 8, 128], DTX); kbg = sb('kbg', [64, 8, 128], DTX); Kd2 = [sb('Kd%d' % i, [64, 8, 128], BF16) for i in range(2)]
        usb2 = [sb('usb%d' % i, [64, 8, 128]) for i in range(2)]; WT2 = [sb('WT%d' % i, [128, 8, 64]) for i in range(2)]; QdT2 = [sb('QdT%d' % i, [128, 8, 64]) for i in range(2)]
        vnew = sb('vnew', [64, 4, 128], BF16)
        Sst = [sb('Sst%d' % i, [128, 4, 128]) for i in range(2)]
        Sb = sb('Sb', [128, 4, 128], BF16)
        kTb = sb('kTb', [128, 4, 128], BF16); qTb = sb('qTb', [128, 4, 128], BF16)
        I64b = I64
        osb = [sb('osb%d' % i, [64, 2, 4, 128]) for i in range(2)]
        P.op('pool', MEMSET(Sst[0][:], 0.0), writes=['Sst0'])
        P.op('pool', MEMSET(Sb[:], 0.0), writes=['Sb'])
        cur = 0
        rev = base == 512
        f2 = lambda t: t[:].rearrange("p a b -> p (a b)")
        def prep_gen(ti, g0, oi):
            b = ti % 2
            egt, QKm, Kd, usb, WT, QdT = egt2[b], QKm2[b], Kd2[b], usb2[b], WT2[b], QdT2[b]
            kegt, kQKm, kKd, kusb, kWT, kQdT = 'egt%d' % b, 'QKm%d' % b, 'Kd%d' % b, 'usb%d' % b, 'WT%d' % b, 'QdT%d' % b
            want_o = oi is not None
            P.op('sp', DMA(kTt[b][:], S['kT'][:, :, g0:g0 + 128].rearrange("h d t -> d h t")), reads=[('kT_s', g0)], writes=['kTt%d' % b], dma=tag + 'kTt%d' % b)
            P.op('sp', DMA(ktk[b][:].rearrange("p c h e -> p c (h e)"), S['ktok'][g0:g0 + 128, :].rearrange("(c p) f -> p c f", p=64)),
                 reads=[('ktok_s', g0)], writes=['ktk%d' % b], dma=tag + 'ktk%d' % b)
            P.op('sp', DMA(vtk[b][:].rearrange("p c h e -> p c (h e)"), S['vtok'][g0:g0 + 128, :].rearrange("(c p) f -> p c f", p=64)),
                 reads=[('vtok_s', g0)], writes=['vtk%d' % b], dma=tag + 'vtk%d' % b)
            P.op('sp', DMA(gts[b][:], S['gate'][g0:g0 + 128, :].rearrange("(c p) f -> p c f", p=64)),
                 reads=[('gate_s', g0)], writes=['gts%d' % b], dma=tag + 'gts%d' % b)
            if want_o:
                P.op('sp', DMA(qTt[b][:], S['qT'][:, :, oi * 128:(oi + 1) * 128].rearrange("h d t -> d h t")), reads=[('qT_s', oi)], writes=['qTt%d' % b], dma=tag + 'qTt%d' % b)
            P.op('pool', CP(kTb[:], kTt[b][:]), reads=['kTt%d' % b], writes=['kTb'])
            if want_o:
                P.op('pool', CP(qTb[:], qTt[b][:]), reads=['qTt%d' % b], writes=['qTb'])
            gk = 'gts%d' % b
            gg = gts[b][:, :, go:go + 4]
            gbeta = gts[b][:, :, 8 + go:12 + go]
            glnb = gts[b][:, :, 16 + go:20 + go]
            ps, pk = self.psum()
            for c in range(2):
                P.op('pe', MM(ps[0:64, 4 * c:4 * c + 4], U, gts[b][:, c, go:go + 4], True, True), reads=['cf', gk], writes=[pk])
                P.op('pe', MM(ps[:, 8 + 4 * c:12 + 4 * c], ones64x128, gts[b][:, c, go:go + 4], True, True), reads=['cf', gk], writes=[pk])
            P.op('act', ACP(gsb[0:64, 0:8], ps[0:64, 0:8]), reads=[pk], writes=['gsb0'])
            P.op('act', ACP(gsb[:, 8:16], ps[:, 8:16]), reads=[pk], writes=['gsb1'])
            P.op('act', ACT(egam[:], gsb[0:64, 0:8], AF.Exp), reads=['gsb0'], writes=['egam'])
            P.op('dve', TT(ekd[:], gsb[0:64, 8:16], gsb[0:64, 0:8], ALU.subtract), reads=['gsb0', 'gsb1'], writes=['ekd'])
            P.op('act', ACT(ekd[:], ekd[:], AF.Exp), reads=['ekd'], writes=['ekd'])
            P.op('act', ACT(egt[:], gsb[:, 8:16], AF.Exp), reads=['gsb1'], writes=[kegt])
            yield
            v4 = lambda t: t[:].rearrange("p (c h) i -> p c h i", c=2)
            Ub = U.unsqueeze(1).unsqueeze(1).to_broadcast([64, 2, 4, 64])
            Ib = I64.unsqueeze(1).unsqueeze(1).to_broadcast([64, 2, 4, 64])
            P.op('dve', TT(v4(G1), Ub, gg.unsqueeze(3).to_broadcast([64, 2, 4, 64]), ALU.mult), reads=['cf', gk], writes=['G1'])
            P.op('pool', TS(f2(G1n), f2(G1), -1.0, ALU.mult), reads=['G1'], writes=['G1n'])
            P.op('pool', TT(v4(G1b), Ib, glnb.unsqueeze(3).to_broadcast([64, 2, 4, 64]), ALU.mult), reads=['cf', gk], writes=['G1b'])
            P.op('pool', TT(f2(G1b), f2(G1b), f2(G1), ALU.add), reads=['G1b', 'G1'], writes=['G1b'])
            yield
            psA, pkA = self.psum()
            psB, pkB = self.psum()
            P.op('pe', MM(psA[0:64, :], ones64, f2(G1b), True, True), reads=['cf', 'G1b'], writes=[pkA])
            P.op('pe', MM(psB[0:64, :], ones64, f2(G1n), True, True), reads=['cf', 'G1n'], writes=[pkB])
            m3 = lambda m: m.unsqueeze(1).to_broadcast([64, 8, 64])
            p3 = lambda p_: p_[0:64, :].rearrange("p (a b) -> p a b", a=8)
            gcol = gsb[0:64, 0:8].unsqueeze(2).to_broadcast([64, 8, 64])
            P.op('dve', TT(gpl[:].rearrange("p (c h) -> p c h", c=2), gsb[0:64, 0:8].rearrange("p (c h) -> p c h", c=2), glnb, ALU.add), reads=['gsb0', gk], writes=['gpl'])
            gplc = gpl[:, :].unsqueeze(2).to_broadcast([64, 8, 64])
            P.op('dve', TT(MaT[:], p3(psA), m3(mbsT), ALU.add), reads=[pkA, 'cf'], writes=['MaT'])
            P.op('dve', TT(MaT[:], MaT[:], gcol, ALU.subtract), reads=['MaT', 'gsb0'], writes=['MaT'])
            P.op('act', ACT(f2(MaT), f2(MaT), AF.Exp), reads=['MaT'], writes=['MaT'])
            P.op('dve', TT(Ma[:], p3(psB), m3(mbs), ALU.add), reads=[pkB, 'cf'], writes=['Ma'])
            P.op('dve', TT(Ma[:], Ma[:], gplc, ALU.add), reads=['Ma', 'gpl'], writes=['Ma'])
            P.op('act', ACT(f2(Ma), f2(Ma), AF.Exp), reads=['Ma'], writes=['Ma'])
            if want_o:
                psC, pkC = self.psum()
                psD, pkD = self.psum()
                P.op('pe', MM(psC[0:64, :], ones64, f2(G1), True, True), reads=['cf', 'G1'], writes=[pkC])
                P.op('pe', MM(psD[:, :], ones64x128, f2(G1), True, True), reads=['cf', 'G1'], writes=[pkD])
                P.op('dve', TT(Dq[:], p3(psC), m3(mbiT), ALU.add), reads=[pkC, 'cf'], writes=['Dq'])
                P.op('dve', TT(Dq[:], Dq[:], gcol, ALU.subtract), reads=['Dq', 'gsb0'], writes=['Dq'])
                P.op('act', ACT(f2(Dq), f2(Dq), AF.Exp), reads=['Dq'], writes=['Dq'])
                P.op('act', ACT(f2(egb), psD[:, :], AF.Exp), reads=[pkD], writes=['egb'])
            yield
            psK, pkK = self.psum()
            for it in range(8):
                c, h = it // 4, it % 4
                sl = slice(it * 64, (it + 1) * 64)
                kc_ = kTb[:, h, c * 64:(c + 1) * 64]
                P.op('pe', MM(psK[0:64, sl], kc_, kc_, True, True), reads=['kTb'], writes=[pkK])
            X0, Y0 = XY[0]
            P.op('dve', TT(f2(X0), psK[0:64, :], f2(MaT), ALU.mult), reads=[pkK, 'MaT'], writes=['X0'])
            P.op('dve', TT(f2(Y0), psK[0:64, :], f2(Ma), ALU.mult), reads=[pkK, 'Ma'], writes=['Y0'])
            if want_o:
                psQ, pkQ = self.psum()
                for it in range(8):
                    c, h = it // 4, it % 4
                    sl = slice(it * 64, (it + 1) * 64)
                    P.op('pe', MM(psQ[0:64, sl], kTb[:, h, c * 64:(c + 1) * 64], qTb[:, h, c * 64:(c + 1) * 64], True, True),
                         reads=['kTb', 'qTb'], writes=[pkQ])
                P.op('dve', TT(f2(QKm), psQ[0:64, :], f2(Dq), ALU.mult), reads=[pkQ, 'Dq'], writes=[kQKm])
                P.op('pool', TT(QdT[:].rearrange("p (c h) i -> p c h i", c=2), qTt[b][:].rearrange("p h (c i) -> p c h i", c=2),
                                egb[:].rearrange("p (c h) i -> p c h i", c=2), ALU.mult), reads=['qTt%d' % b, 'egb'], writes=[kQdT])
            yield
            P.op('pool', TT(R[:], m3(I64), X0[:], ALU.subtract), reads=['cf', 'X0'], writes=['R'])
            pp = 0
            for lvl in range(5):
                Xc, Yc = XY[pp]
                Xn, Yn = XY[1 - pp]
                kx, ky, nx, ny = 'X%d' % pp, 'Y%d' % pp, 'X%d' % (1 - pp), 'Y%d' % (1 - pp)
                psY, pkY = self.psum()
                for it in range(8):
                    sl = slice(it * 64, (it + 1) * 64)
                    P.op('pe', MM(psY[0:64, sl], Xc[:, it, :], Yc[:, it, :], True, True), reads=[kx, ky], writes=[pkY])
                P.op('act', ACP(f2(Yn), psY[0:64, :]), reads=[pkY], writes=[ny])
                if lvl < 4:
                    psX, pkX = self.psum()
                    for it in range(8):
                        sl = slice(it * 64, (it + 1) * 64)
                        P.op('pe', MM(psX[0:64, sl], Yc[:, it, :], Xc[:, it, :], True, True), reads=[kx, ky], writes=[pkX])
                    P.op('act', ACP(f2(Xn), psX[0:64, :]), reads=[pkX], writes=[nx])
                psR, pkR = self.psum()
                for it in range(8):
                    sl = slice(it * 64, (it + 1) * 64)
                    P.op('pe', MM(psR[0:64, sl], Yn[:, it, :], R[:, it, :], True, True), reads=[ny, 'R'], writes=[pkR])
                P.op('dve', TT(f2(R), f2(R), psR[0:64, :], ALU.add), reads=[pkR, 'R'], writes=['R'])
                pp = 1 - pp
                yield
            yield
            b4 = lambda a: a.unsqueeze(3).to_broadcast([64, 2, 4, 128])
            e4 = lambda t: t[:].rearrange("p (c h) e -> p c h e", c=2)
            P.op('dve', TT(bege[:].rearrange("p (c h) -> p c h", c=2), gbeta, egam[:].rearrange("p (c h) -> p c h", c=2), ALU.mult), reads=[gk, 'egam'], writes=['bege'])
            P.op('pool', TT(e4(vb), vtk[b][:], b4(gbeta), ALU.mult), reads=['vtk%d' % b, gk], writes=['vb'])
            P.op('pool', TT(e4(kbg), ktk[b][:], b4(bege[:].rearrange("p (c h) -> p c h", c=2)), ALU.mult), reads=['ktk%d' % b, 'bege'], writes=['kbg'])
            P.op('pool', TT(e4(Kd), ktk[b][:], b4(ekd[:].rearrange("p (c h) -> p c h", c=2)), ALU.mult), reads=['ktk%d' % b, 'ekd'], writes=[kKd])
            yield
            for c in range(2):
                psU, pkU = self.psum()
                for h in range(4):
                    it = c * 4 + h
                    P.op('pe', MM(psU[0:64, h * 128:(h + 1) * 128], R[:, it, :], vb[:, it, :], True, True), reads=['R', 'vb'], writes=[pkU])
                P.op('act', ACP(usb[:, 4 * c:4 * c + 4, :].rearrange("p a b -> p (a b)"), psU[0:64, :]), reads=[pkU], writes=[(kusb, c)])
            psW, pkW = self.psum()
            for it in range(8):
                sl = slice(it * 64, (it + 1) * 64)
                P.op('pe', MM(psW[:, sl], kbg[:, it, :], R[:, it, :], True, True), reads=['R', 'kbg'], writes=[pkW])
            P.op('act', ACP(f2(WT), psW[:, :]), reads=[pkW], writes=[kWT])
            yield

        def scan_gen(ti, g0, oi):
            nonlocal cur
            b = ti % 2
            want_o = oi is not None
            egt, QKm, Kd, usb, WT, QdT = egt2[b], QKm2[b], Kd2[b], usb2[b], WT2[b], QdT2[b]
            kegt, kQKm, kKd, kusb, kWT, kQdT = 'egt%d' % b, 'QKm%d' % b, 'Kd%d' % b, 'usb%d' % b, 'WT%d' % b, 'QdT%d' % b
            ob = ti % 2
            for c in ((1, 0) if rev else (0, 1)):
                So, Sn = Sst[cur], Sst[1 - cur]
                ko, kn_ = 'Sst%d' % cur, 'Sst%d' % (1 - cur)
                psS, pkS = self.psum()
                for h in range(4):
                    P.op('pe', MM(psS[0:64, h * 128:(h + 1) * 128], WT[:, 4 * c + h, :], So[:, h, :], True, True), reads=[kWT, ko], writes=[pkS])
                P.op('dve', TT(f2(vnew), usb[:, 4 * c:4 * c + 4, :].rearrange("p a b -> p (a b)"), psS[0:64, :], ALU.subtract),
                     reads=[pkS, (kusb, c)], writes=['vnew'])
                if want_o:
                    psO, pkO = self.psum()
                    for h in range(4):
                        it = 4 * c + h
                        P.op('pe', MM(psO[0:64, h * 128:(h + 1) * 128], QdT[:, it, :], So[:, h, :], True, False), reads=[kQdT, ko], writes=[pkO])
                        P.op('pe', MM(psO[0:64, h * 128:(h + 1) * 128], QKm[:, it, :], vnew[:, h, :], False, True), reads=[kQKm, 'vnew'], writes=[pkO])
                    P.op('act', ACP(osb[ob][:, c, :, :].rearrange("p a b -> p (a b)"), psO[0:64, :]), reads=[pkO], writes=[('osb', ob, c)])
                yield
                psD2, pkD2 = self.psum()
                for h in range(4):
                    P.op('pe', MM(psD2[:, h * 128:(h + 1) * 128], Kd[:, 4 * c + h, :], vnew[:, h, :], True, True), reads=[kKd, 'vnew'], writes=[pkD2])
                for h in range(4):
                    P.op('dve', STT(Sn[:, h, :], So[:, h, :], egt[:, 4 * c + h:4 * c + h + 1], psD2[:, h * 128:(h + 1) * 128], ALU.mult, ALU.add),
                         reads=[ko, kegt, pkD2], writes=[(kn_, h)] if False else [kn_])
                cur = 1 - cur
                yield
            if want_o:
                P.op('pool', DMA(out_ap[oi * 128:(oi + 1) * 128, :].rearrange("(c p) f -> p c f", p=64), osb[ob][:].rearrange("p c h e -> p c (h e)")),
                     reads=[('osb', ob, 0), ('osb', ob, 1)], writes=[(tag + 'o_s', oi)], dma=tag + 'osb%d' % ob)
            yield

        def drain(*gens):
            gens = [x for x in gens if x is not None]
            while gens:
                for x in list(gens):
                    try:
                        next(x)
                    except StopIteration:
                        gens.remove(x)
        prev = None
        for ti, (g0, oi) in enumerate(tiles):
            drain(prep_gen(ti, g0, oi), prev)
            prev = scan_gen(ti, g0, oi)
            if extra is not None:
                next(extra, None)
                next(extra, None)
        drain(prev)
        if extra is not None:
            for _ in extra:
                pass
        if 'state' in self.dbg:
            P.op('pool', DMA(self.dbg_state[tag], Sst[cur][:].rearrange("p a b -> p (a b)")), reads=['Sst%d' % cur], dma=tag + 'dbgS')
        P.barrier()
        es.close()

    def phaseG(self):
        NT, NL = self.NT, self.NL
        tilesA = [(0, None), (128, None)] + [(256 + i * 128, i) for i in range(NT)]
        esT = ExitStack()
        self.gdn_pass('ga_', 256, 0, tilesA, self.S['oA'], extra=self.t_steps(esT))
        esT.close()
        if self.stop_after == 'GA':
            return
        tilesB = [(128, None), (0, None)] + [(256 + i * 128, (i if i < NT else None)) for i in range(NL - 1, -1, -1)]
        self.gdn_pass('gb_', 512, 4, tilesB, self.S['oB'])


def _consts():
    cf = np.zeros((128, 1280), np.float32)
    cf[:, 0:128] = np.eye(128, dtype=np.float32)
    cf[:, 128:256] = 1.0
    t = np.arange(64)
    UF = (t[:, None] <= t[None, :]).astype(np.float32)
    UB = (t[:, None] >= t[None, :]).astype(np.float32)
    col = 256
    for U in (UF, UB):
        cf[0:64, col:col + 64] = U
        cf[0:64, col + 64:col + 128] = (U - 1.0) * BIG
        strict = U - np.eye(64, dtype=np.float32)
        cf[0:64, col + 128:col + 192] = (strict - 1.0) * BIG
        cf[0:64, col + 192:col + 256] = (strict.T - 1.0) * BIG
        col += 256
    cf[0:64, 768:832] = np.eye(64, dtype=np.float32)
    cf[:, 832:848] = np.arange(16, dtype=np.float32)[None, :]
    import ml_dtypes
    cb = np.zeros((128, 512), np.float32)
    cb[:, 0:128] = np.eye(128)
    k = np.arange(128)
    cb[:, 128:256] = (k[:, None] >= k[None, :])
    cb[:, 256:384] = (k[:, None] <= k[None, :])
    cb[:, 384:512] = 1.0
    return cf, cb.astype(ml_dtypes.bfloat16)


def prepare_inputs(inp, NT, cores=range(8)):
    NL = 2 * NT
    seq = NL * 128
    f32 = lambda a: np.ascontiguousarray(a, dtype=np.float32)
    w_in = inp['w_in'][0]
    qa, ka, va, dq, dk, dv, z, gb = np.split(w_in, [512, 640, 768, 1280, 1792, 2304, 2816], axis=1)

    def perm_rope(w, nh):
        w4 = w.reshape(D, nh, 2, 2, 16)
        return w4[:, :, :, ::-1, :].reshape(D, nh * 64)
    cf, cb = _consts()
    nf = 16
    freqs = (10000.0 ** (-np.arange(nf, dtype=np.float32) / nf)).astype(np.float32)
    maps = []
    for c in cores:
        b, half = c // 2, c % 2
        x = inp['x'][b, :seq]
        ctx = inp['ctx'][b]
        gbl = gb if half == 0 else gb[:, [4, 5, 6, 7, 0, 1, 2, 3, 12, 13, 14, 15, 8, 9, 10, 11]]
        conv = inp['conv_w'][0]
        alog, dtb = inp['a_log'][0], inp['dt_bias'][0]
        if half == 1:
            x, ctx, conv = x[::-1], ctx[::-1], conv[::-1]
            alog, dtb = alog[::-1], dtb[::-1]
        wext = np.concatenate([qa, perm_rope(qa, 8), ka, perm_rope(ka, 2), dq, dk, dv, z, va, gbl], axis=1)
        assert wext.shape[1] == WEXT
        cvec = np.concatenate([inp['c'][b].reshape(8, 128).T, inp['c_ctx'].reshape(8, 128).T], axis=1)
        ntab = (NT + 1) * 128
        tau = np.arange(ntab)
        pos = tau if half == 0 else (seq - 1 - tau)
        row = (pos // 64).astype(np.float32)
        colp = (pos % 64).astype(np.float32)
        ropeC = np.zeros((64, ntab), np.float32)
        ropeS = np.zeros((64, ntab), np.float32)
        for hb_, p_ in ((0, row), (32, colp)):
            ang = (p_[:, None] * freqs[None, :]).astype(np.float32)
            cs_, sn_ = np.cos(ang).astype(np.float32).T, np.sin(ang).astype(np.float32).T
            ropeC[hb_:hb_ + 16] = cs_
            ropeC[hb_ + 16:hb_ + 32] = cs_
            ropeS[hb_:hb_ + 16] = -sn_
            ropeS[hb_ + 16:hb_ + 32] = sn_
        m = {
            'x': f32(x), 'ctx': f32(ctx), 'cvec': f32(cvec),
            'w_ada': f32(inp['w_ada'][0]), 'b_ada': f32(inp['b_ada'][0].reshape(1, -1)),
            'w_in': f32(wext),
            'cw': f32(conv.reshape(5, 12, 128).transpose(2, 1, 0)),
            'adt': f32(np.concatenate([alog.reshape(-1), dtb.reshape(-1)]).reshape(1, 16)),
            'sink': f32(inp['sink'][0].reshape(1, 8)), 'wnorm': f32(np.tile(inp['dn_norm_w'][0].reshape(1, 128), (128, 4))),
            'w_out': f32(inp['w_out'][0]),
            'lnp': f32(np.tile(np.concatenate([inp['ln1_g'][0], inp['ln1_b'][0], inp['ln2_g'][0], inp['ln2_b'][0]]).reshape(1, -1), (128, 1))),
            'wq': f32(inp['peer_wq'][0]),
            'skT': f32(inp['peer_sub_keys'][0].transpose(2, 0, 1)),
            'peer_u': f32(inp['peer_u'][0]), 'peer_v': f32(inp['peer_v'][0]),
            'ropeC': ropeC, 'ropeS': ropeS, 'cf': cf, 'cb': cb,
        }
        maps.append(m)
    return maps


_NC_CACHE = {}


def kernel(**inputs):
    NT = 32
    if NT not in _NC_CACHE:
        _NC_CACHE[NT] = Builder(NT).build()
    nc = _NC_CACHE[NT]
    maps = prepare_inputs(inputs, NT)
    res = run_bass_kernel_spmd(nc, maps, core_ids=list(range(8)))
    out = np.zeros((4, 8192, D), np.float32)
    for c in range(8):
        b, half = c // 2, c % 2
        o = np.asarray(res.results[c]['out'])
        if half == 0:
            out[b, :4096] = o
        else:
            out[b, 4096:] = o[::-1]
    return out
```

```python
import numpy as np
from contextlib import ExitStack
import concourse.bass as bass
import concourse.mybir as mybir
from concourse.bass_utils import run_bass_kernel_spmd

F32 = mybir.dt.float32
BF16 = mybir.dt.bfloat16
I32 = mybir.dt.int32
U32 = mybir.dt.uint32
AF = mybir.ActivationFunctionType
ALU = mybir.AluOpType
AX = mybir.AxisListType

ENGS = ['pe', 'act', 'dve', 'pool', 'sp']

D = 1024
QA, QAP, KA, KAP, DQ, DK, DV, ZC, VA, GB, WEXT = 0, 512, 1024, 1152, 1280, 1792, 2304, 2816, 3328, 3456, 3472
EPS = 1e-6
ALPHA = 2.0 ** 0.25
BIG = 30000.0


class Prog:
    def __init__(self, nc, es):
        self.nc = nc
        self.es = es
        self.recs = {e: [] for e in ENGS}
        self.cnt = {e: 0 for e in ENGS}
        self.known = {e: {} for e in ENGS}
        self.state = {}
        self.esem = {e: es.enter_context(nc.semaphore('s_' + e)) for e in ENGS if e != 'sp'}
        self.dsem = {}
        self.dcnt = {}
        self.tokinfo = {}
        self.nwaits = 0
        self.free_sems = []
        self.all_sems = []
        self.semcnt = {}
        self.uniq = 0

    def dma_sem(self, name):
        if name not in self.dsem:
            if self.free_sems:
                sem = self.free_sems.pop()
            else:
                sem = self.es.enter_context(self.nc.semaphore('d_%d' % len(self.all_sems)))
                self.all_sems.append(sem)
                self.semcnt[sem] = 0
            self.dsem[name] = sem
        return name

    def op(self, eng, fn, reads=(), writes=(), dma=None):
        waits = {}
        kn = self.known[eng]
        own = self.esem.get(eng)

        def need(tok):
            sem, val = tok
            if eng == 'pe' and sem is own:
                return
            if kn.get(sem, 0) >= val:
                return
            if waits.get(sem, 0) < val:
                waits[sem] = val

        for k in reads:
            st = self.state.get(k)
            if st is not None and st[0] is not None:
                need(st[0])
        for k in writes:
            st = self.state.get(k)
            if st is not None:
                if st[0] is not None:
                    need(st[0])
                for r in st[1]:
                    need(r)
        for sem, val in waits.items():
            kn[sem] = val
            info = self.tokinfo.get((sem, val))
            if info:
                for s2, v2 in info.items():
                    if kn.get(s2, 0) < v2:
                        kn[s2] = v2
        if dma is None:
            self.cnt[eng] += 1
            tok = (own, self.cnt[eng])
            inc = (own, 1)
        else:
            if dma == '*':
                self.uniq += 1
                dma = '*%d' % self.uniq
            self.dma_sem(dma)
            sem = self.dsem[dma]
            self.semcnt[sem] += 16
            tok = (sem, self.semcnt[sem])
            inc = (sem, 16)
        self.tokinfo[tok] = dict(kn)
        for k in reads:
            st = self.state.get(k)
            if st is None:
                self.state[k] = [None, [tok]]
            else:
                st[1].append(tok)
        for k in writes:
            self.state[k] = [tok, []]
        self.nwaits += len(waits)
        self.recs[eng].append((list(waits.items()), fn, inc))
        return tok

    def barrier(self):
        toks = []
        for e in ENGS:
            if e != 'sp' and self.cnt[e] > 0:
                toks.append((self.esem[e], self.cnt[e]))
        for sem in self.all_sems:
            if self.semcnt[sem] > 0:
                toks.append((sem, self.semcnt[sem]))
        for e in ENGS:
            kn = self.known[e]
            w = []
            for sem, val in toks:
                if kn.get(sem, 0) < val:
                    kn[sem] = val
                    w.append((sem, val))
            if w:
                self.recs[e].append((w, None, None))
        self.state = {}
        self.tokinfo = {}
        self.free_sems = list(self.all_sems)
        self.dsem = {}

    def emit(self):
        nc = self.nc
        self.barrier()
        with nc.Block() as block:
            def run(name):
                def f(e):
                    for waits, fn, inc in self.recs[name]:
                        for sem, val in waits:
                            e.wait_ge(sem, val)
                        if fn is not None:
                            ins = fn(e)
                            ins.then_inc(inc[0], inc[1])
                return f
            block.tensor(run('pe'))
            block.scalar(run('act'))
            block.vector(run('dve'))
            block.gpsimd(run('pool'))
            block.sync(run('sp'))


def MM(out, lhsT, rhs, st, sp):
    return lambda e: e.matmul(out, lhsT=lhsT, rhs=rhs, start=st, stop=sp)


def TR(out, in_, ident):
    return lambda e: e.transpose(out=out, in_=in_, identity=ident)


def ACT(out, in_, func, scale=1.0, bias=None, accum=None):
    kw = {}
    if bias is not None:
        kw['bias'] = bias
    if accum is not None:
        kw['accum_out'] = accum
    return lambda e: e.activation(out=out, in_=in_, func=func, scale=scale, **kw)


def ACP(out, in_):
    return lambda e: e.copy(out=out, in_=in_)


def TT(out, a, b, op):
    return lambda e: e.tensor_tensor(out=out, in0=a, in1=b, op=op)


def TS(out, a, s1, op0, s2=None, op1=None, accum=None):
    kw = {}
    if op1 is not None:
        kw['op1'] = op1
    if accum is not None:
        kw['accum_out'] = accum
    return lambda e: e.tensor_scalar(out=out, in0=a, scalar1=s1, scalar2=s2, op0=op0, **kw)


def STT(out, a, s, b, op0, op1, accum=None):
    kw = {}
    if accum is not None:
        kw['accum_out'] = accum
    return lambda e: e.scalar_tensor_tensor(out=out, in0=a, scalar=s, in1=b, op0=op0, op1=op1, **kw)


def CP(out, in_):
    return lambda e: e.tensor_copy(out=out, in_=in_)


def MEMSET(out, v):
    return lambda e: e.memset(out, v)


def DMA(out, in_):
    return lambda e: e.dma_start(out=out, in_=in_)


def RECIP(out, in_):
    return lambda e: e.reciprocal(out=out, in_=in_)


class Builder:
    def __init__(self, NT, dbg=(), stop_after=None):
        self.NT = NT
        self.dbg = set(dbg)
        self.stop_after = stop_after
        self.nc = bass.Bass("TRN2", target_bir_lowering=False)
        self.NL = 2 * NT
        self.NG = 256 + self.NL * 128
        self.NO = NT * 128

    def din(self, name, shape, dt=F32):
        return self.nc.dram_tensor(name, list(shape), dt, kind="ExternalInput").ap()

    def dout(self, name, shape, dt=F32):
        return self.nc.dram_tensor(name, list(shape), dt, kind="ExternalOutput").ap()

    def dscr(self, name, shape, dt=F32):
        kind = "ExternalOutput" if self.dbg else "Internal"
        return self.nc.dram_tensor(name, list(shape), dt, kind=kind).ap()

    def build(self):
        nc = self.nc
        NT, NL, NG, NO = self.NT, self.NL, self.NG, self.NO
        I = {}
        I['x'] = self.din('x', [NL * 128, D])
        I['ctx'] = self.din('ctx', [256, D])
        I['cvec'] = self.din('cvec', [128, 16])
        I['w_ada'] = self.din('w_ada', [D, 6 * D])
        I['b_ada'] = self.din('b_ada', [1, 6 * D])
        I['w_in'] = self.din('w_in', [D, WEXT])
        I['cw'] = self.din('cw', [128, 12, 5])
        I['adt'] = self.din('adt', [1, 16])
        I['sink'] = self.din('sink', [1, 8])
        I['wnorm'] = self.din('wnorm', [128, 512])
        I['w_out'] = self.din('w_out', [D, D])
        I['lnp'] = self.din('lnp', [128, 4 * D])
        I['wq'] = self.din('wq', [D, 2048])
        I['skT'] = self.din('skT', [128, 2, 128])
        I['peer_u'] = self.din('peer_u', [16384, D])
        I['peer_v'] = self.din('peer_v', [16384, D])
        I['ropeC'] = self.din('ropeC', [64, (NT + 1) * 128])
        I['ropeS'] = self.din('ropeS', [64, (NT + 1) * 128])
        I['cf'] = self.din('cf', [128, 1280])
        I['cb'] = self.din('cb', [128, 512], BF16)
        self.I = I
        self.out = self.dout('out', [NO, D])
        S = {}
        S['kT'] = self.dscr('kT_s', [4, 128, NG])
        S['qT'] = self.dscr('qT_s', [4, 128, NO])
        S['ktok'] = self.dscr('ktok_s', [NG, 512])
        S['vtok'] = self.dscr('vtok_s', [NG, 512])
        S['gate'] = self.dscr('gate_s', [NG, 24])
        S['zs'] = self.dscr('zs_s', [NO, 512])
        S['attnT'] = self.dscr('attnT_s', [64, 8, NO], BF16)
        S['oA'] = self.dscr('oA_s', [NO, 512])
        S['x1'] = self.dscr('x1_s', [NO, D])
        S['oB'] = self.dscr('oB_s', [NO, 512])
        S['uv'] = self.dscr('uv_s', [16384, 2 * D], BF16)
        self.dbg_state = {}
        if 'state' in self.dbg:
            self.dbg_state = {t: self.dout('dbgS_' + t, [128, 512]) for t in ('ga_', 'gb_')}
        self.S = S
        self.dbg_out = {}
        with ExitStack() as es:
            self.es = es
            self.P = Prog(nc, es)
            self.phase0()
            if self.stop_after not in ('p0', 'p0a', 'p0b', 'p0c', 'p0d', 'p0e'):
                self.phaseA()
                if self.stop_after not in ('A',):
                    self.phaseG()
                    if self.stop_after not in ('GA', 'G'):
                        self.phaseM()
                        if self.stop_after not in ('M',):
                            self.phaseC()
            self.P.emit()
        return nc

    def sb(self, name, shape, dt=F32, es=None):
        return (es or self.es).enter_context(self.nc.sbuf_tensor('sb_' + name, list(shape), dt))

    def pst(self, name, shape, dt=F32, es=None):
        return (es or self.es).enter_context(self.nc.psum_tensor('pp_' + name, list(shape), dt))

    def psum(self):
        lim = getattr(self, 'ps_lim', len(self.psf))
        i = self.ps_next % lim
        self.ps_next = (i + 1) % lim
        return self.psf[i], 'ps%d' % i

    def dump(self, name, ap_sb, shape, reads, dt=F32):
        if name not in self.dbg_out:
            self.dbg_out[name] = self.dout('dbg_' + name, shape, dt)
        return self.dbg_out[name]

    def phase0(self):
        P, I, nc = self.P, self.I, self.nc
        sb = self.sb
        self.psf = [self.pst('psf%d' % i, [128, 512]) for i in range(7)]
        self.psb = self.pst('psb', [128, 1024], BF16)
        self.ps_next = 0
        self.cf = sb('cf', [128, 1280])
        self.cb = sb('cb', [128, 512], BF16)
        P.op('sp', DMA(self.cf[:], I['cf'][:, :]), writes=['cf'], dma='*')
        P.op('sp', DMA(self.cb[:], I['cb'][:, :]), writes=['cb'], dma='*')
        self.ident_f = self.cf[:, 0:128]
        self.ones_f = self.cf[:, 128:256]
        self.ident_b = self.cb[:, 0:128]
        self.maskL = self.cb[:, 128:256]
        self.maskR = self.cb[:, 256:384]
        self.ones_b = self.cb[:, 384:512]
        self.epsc = sb('epsc', [128, 4])
        P.op('pool', MEMSET(self.epsc[:, 0:1], EPS), writes=['epsc0'])
        P.op('pool', MEMSET(self.epsc[:, 1:2], 128 * EPS), writes=['epsc1'])
        P.op('pool', MEMSET(self.epsc[:, 2:3], 1.0), writes=['epsc2'])
        self.EPSK = ['epsc0', 'epsc1', 'epsc2']
        if self.stop_after == 'p0a':
            return
        self.modb = sb('modb', [128, 6 * D])
        self.esA = ExitStack()
        sbA = lambda name, shape, dt=F32: self.sb(name, shape, dt, es=self.esA)
        self.modc = sbA('modc', [128, 2 * D])
        self.w_in = sbA('w_in_sb', [128, 8, WEXT], BF16)
        self.cw = sbA('cw', [128, 12, 5])
        adt = sbA('adt', [128, 16])
        self.nexp = sbA('nexp', [128, 8])
        sk = sbA('sk', [64, 8])
        self.esink = sbA('esink', [64, 8, 128])
        self.wnbc = sbA('wnbc', [128, 4, 128])
        es2 = ExitStack()
        sb2 = lambda name, shape, dt=F32: self.sb(name, shape, dt, es=es2)
        cv = sb2('cv', [128, 16])
        P.op('sp', DMA(cv[:], I['cvec'][:, :]), writes=['cv'], dma='*')
        cs_ = sb2('cs_', [128, 16])
        P.op('act', ACT(cs_[:], cv[:], AF.Silu), reads=['cv'], writes=['cs_'])
        crep = sb2('crep', [128, 16, 128])
        P.op('dve', CP(crep[:], cs_[:, :].unsqueeze(2).to_broadcast([128, 16, 128])), reads=['cs_'], writes=['crep'])
        bada = sb2('bada', [1, 6 * D])
        P.op('sp', DMA(bada[:], I['b_ada'][:, :]), writes=['bada'], dma='*')
        wst = [self.sb('wst%d' % i, [128, 8, 512], es=es2) for i in range(2)]
        wa = I['w_ada'].rearrange("(kc p) n -> p kc n", p=128)
        for n in range(12):
            s = n % 2
            P.op('sp', DMA(wst[s][:], wa[:, :, n * 512:(n + 1) * 512]), writes=['wst%d' % s], dma='wst%d' % s)
            variants = [(0, self.modb)] + ([(8, self.modc)] if n < 4 else [])
            for off, dst in variants:
                ps, pk = self.psum()
                for kc in range(8):
                    P.op('pe', MM(ps[:, :], crep[:, off + kc, :], wst[s][:, kc, :], kc == 0, False),
                         reads=['crep', 'wst%d' % s], writes=[pk])
                P.op('pe', MM(ps[:, :], self.ones_f[0:1, :], bada[0:1, n * 512:(n + 1) * 512], False, True),
                     reads=['cf', 'bada'], writes=[pk])
                dk = ('mod', id(dst), n)
                if n in (2, 3, 8, 9):
                    P.op('dve', TS(dst[:, n * 512:(n + 1) * 512], ps[:, :], 1.0, ALU.add), reads=[pk], writes=[dk])
                else:
                    P.op('act', ACP(dst[:, n * 512:(n + 1) * 512], ps[:, :]), reads=[pk], writes=[dk])
        self.MODK = [('mod', id(self.modb), n) for n in range(12)]
        self.MODCK = [('mod', id(self.modc), n) for n in range(4)]
        P.barrier()
        es2.close()
        if self.stop_after == 'p0b':
            return
        for kc in range(8):
            for (a, b) in ((0, 1736), (1736, WEXT)):
                P.op('pool', DMA(self.w_in[:, kc, a:b], I['w_in'][kc * 128:(kc + 1) * 128, a:b]),
                     writes=[('w_in', kc, a)], dma='w_in')
        self.WINK = [('w_in', kc, a) for kc in range(8) for a in (0, 1736)]
        if self.stop_after == 'p0c':
            return
        P.op('sp', DMA(self.cw[:], I['cw'][:, :, :]), writes=['cw'], dma='*')
        P.op('sp', DMA(adt[:], I['adt'].partition_broadcast(128)), writes=['adt'], dma='*')
        P.op('act', ACT(self.nexp[:], adt[:, 0:8], AF.Exp), reads=['adt'], writes=['nexp'])
        P.op('dve', TS(self.nexp[:], self.nexp[:], -1.0, ALU.mult), reads=['nexp'], writes=['nexp'])
        self.dtb = adt[:, 8:16]
        if self.stop_after == 'p0d':
            return
        P.op('sp', DMA(sk[:], I['sink'].partition_broadcast(64)), writes=['sk'], dma='*')
        P.op('act', ACT(sk[:], sk[:], AF.Exp), reads=['sk'], writes=['sk'])
        P.op('dve', CP(self.esink[:], sk[:, :].unsqueeze(2).to_broadcast([64, 8, 128])), reads=['sk'], writes=['esink'])
        if self.stop_after == 'p0e':
            return
        P.op('sp', DMA(self.wnbc[:].rearrange("p a b -> p (a b)"), I['wnorm'][:, :]), writes=['wnbc'], dma='*')

    def phaseA(self):
        P, I, S, nc = self.P, self.I, self.S, self.nc
        NT, NL = self.NT, self.NL
        es = ExitStack()
        sb = lambda name, shape, dt=F32: self.sb(name, shape, dt, es=es)
        xt = [sb('xt%d' % i, [128, D]) for i in range(2)]
        xn = sb('xn', [128, D])
        hb = [sb('hb%d' % i, [128, D], BF16) for i in range(2)]
        hT = [sb('hT%d' % i, [128, 8, 128], BF16) for i in range(2)]
        st6 = sb('st6', [128, 2, 6])
        mv = sb('mv', [128, 2])
        rstd = sb('rstd', [128, 1])
        pad = [sb('pad%d' % i, [128, 12, 132]) for i in range(3)]
        cs = [sb('cs%d' % i, [128, 12, 128]) for i in range(2)]
        sq = sb('sq', [128, 8, 128])
        rn = sb('rn', [128, 8, 128])
        rC = [sb('rC%d' % i, [64, 128]) for i in range(2)]
        rS = [sb('rS%d' % i, [64, 128]) for i in range(2)]
        rt1 = sb('rt1', [64, 2, 128])
        rt2 = sb('rt2', [64, 2, 128])
        kTr = [sb('kTr%d' % i, [64, 2, 128], BF16) for i in range(4)]
        vr = [sb('vr%d' % i, [128, 2, 64], BF16) for i in range(4)]
        qTr = [sb('qTr%d' % i, [64, 8, 128], BF16) for i in range(2)]
        kTc = sb('kTc', [64, 2, 2, 128], BF16)
        vc = sb('vc', [128, 2, 2, 64], BF16)
        pT = [sb('pT%d' % i, [128, 512], BF16) for i in range(10)]
        zt = sb('zt', [64, 512])
        attT = [sb('attT%d' % i, [64, 8, 128], BF16) for i in range(2)]
        tok = [sb('tok%d' % i, [128, 512]) for i in range(2)]
        zsb = [sb('zsb%d' % i, [128, 512]) for i in range(2)]
        gt = sb('gt', [128, 16])
        gbs = sb('gbs', [128, 16])
        gl = sb('gl', [128, 16])
        gst = [sb('gst%d' % i, [128, 24]) for i in range(2)]
        w_in = self.w_in
        cnt = {'x': 0, 'pT': 0, 'cs': 0, 'tok': 0, 'att': 0}

        def project(kind, idx, si):
            own = kind == 'loc' and idx < NT
            need_q = kind == 'loc' and idx <= NT
            need_akv = kind == 'ctx' or (kind == 'loc' and idx <= NT)
            s = cnt['x'] % 2
            cnt['x'] += 1
            src = I['ctx'] if kind == 'ctx' else I['x']
            P.op('sp', DMA(xt[s][:], src[idx * 128:(idx + 1) * 128, :]), writes=['xt%d' % s], dma='xt%d' % s)
            for hh in range(2):
                P.op('dve', lambda e, hh=hh, s=s: e.bn_stats(out=st6[:, hh, :], in_=xt[s][:, hh * 512:(hh + 1) * 512]),
                     reads=['xt%d' % s], writes=[('st6', hh)])
            P.op('dve', lambda e: e.bn_aggr(out=mv[:], in_=st6[:].rearrange("p a b -> p (a b)")), reads=[('st6', 0), ('st6', 1)], writes=['mv'])
            P.op('act', ACT(rstd[:], mv[:, 1:2], AF.Ln, bias=self.epsc[:, 0:1]), reads=['mv', 'epsc0'], writes=['rstd'])
            P.op('act', ACT(rstd[:], rstd[:], AF.Exp, scale=-0.5), reads=['rstd'], writes=['rstd'])
            P.op('dve', TS(xn[:], xt[s][:], mv[:, 0:1], ALU.subtract, rstd[:, 0:1], ALU.mult),
                 reads=['xt%d' % s, 'mv', 'rstd'], writes=['xn'])
            if kind == 'ctx':
                sh, sc, mk = self.modc[:, 0:D], self.modc[:, D:2 * D], self.MODCK
            else:
                sh, sc, mk = self.modb[:, 0:D], self.modb[:, D:2 * D], self.MODK[0:4]
            P.op('pool', TT(xn[:], xn[:], sc, ALU.mult), reads=['xn'] + mk, writes=['xn'])
            P.op('dve', TT(hb[s][:], xn[:], sh, ALU.add), reads=['xn'] + mk, writes=['hb%d' % s])
            for kc in range(8):
                P.op('pe', TR(self.psb[:, kc * 128:(kc + 1) * 128], hb[s][:, kc * 128:(kc + 1) * 128], self.ident_b),
                     reads=['hb%d' % s, 'cb'], writes=['psb'])
            P.op('act', ACP(hT[s][:].rearrange("p a b -> p (a b)"), self.psb[:, :]), reads=['psb'], writes=['hT%d' % s])
            hk = 'hT%d' % s
            if self.stop_after == 'projA':
                return

            def fm_group(ps, pk, specs):
                for (o, c0, M) in specs:
                    for kc in range(8):
                        P.op('pe', MM(o, w_in[:, kc, c0:c0 + M], hT[s][:, kc, :], kc == 0, kc == 7),
                             reads=[hk] + self.WINK, writes=[pk])

            if kind == 'loc' and idx <= NT:
                r = idx % 2
                P.op('sp', DMA(rC[r][:], I['ropeC'][:, idx * 128:(idx + 1) * 128]), writes=['rC%d' % r], dma='rC%d' % r)
                P.op('sp', DMA(rS[r][:], I['ropeS'][:, idx * 128:(idx + 1) * 128]), writes=['rS%d' % r], dma='rS%d' % r)
                Cb = rC[r][:, :].unsqueeze(1).to_broadcast([64, 2, 128])
                Sb = rS[r][:, :].unsqueeze(1).to_broadcast([64, 2, 128])
                groups = []
                if own:
                    for p in range(4):
                        groups.append(('q', p))
                groups.append(('k', 0))
                for (typ, p) in groups:
                    ps, pk = self.psum()
                    psv = ps[0:64, :].rearrange("p (h x t) -> p h x t", h=2, x=2)
                    specs = []
                    for hh in range(2):
                        hd = 2 * p + hh
                        c_x = (QA if typ == 'q' else KA) + 64 * hd
                        c_p = (QAP if typ == 'q' else KAP) + 64 * hd
                        specs.append((psv[:, hh, 0, :], c_x, 64))
                        specs.append((psv[:, hh, 1, :], c_p, 64))
                    fm_group(ps, pk, specs)
                    P.op('dve', TT(rt1[:], psv[:, :, 0, :], Cb, ALU.mult), reads=[pk, 'rC%d' % r], writes=['rt1'])
                    P.op('dve', TT(rt2[:], psv[:, :, 1, :], Sb, ALU.mult), reads=[pk, 'rS%d' % r], writes=['rt2'])
                    if typ == 'q':
                        dst, dk_ = qTr[idx % 2][:, 2 * p:2 * p + 2, :], ('qTr', idx % 2, p)
                    else:
                        dst, dk_ = kTr[idx % 4][:, :, :], 'kTr%d' % (idx % 4)
                    P.op('pool', TT(dst, rt1[:], rt2[:], ALU.add), reads=['rt1', 'rt2'], writes=[dk_])
            elif kind == 'ctx':
                ps, pk = self.psum()
                psv = ps[0:64, 0:256].rearrange("p (h t) -> p h t", h=2)
                fm_group(ps, pk, [(psv[:, hh, :], KA + 64 * hh, 64) for hh in range(2)])
                P.op('act', ACP(kTc[:, idx, :, :], psv), reads=[pk], writes=[('kTc', idx)])
            if self.stop_after == 'projB':
                return
            ps_i = si % 3
            chunks = list(range(12)) if need_q else list(range(4, 12))
            for g0 in range(0, 12, 4):
                js = [j for j in chunks if g0 <= j < g0 + 4]
                if not js:
                    continue
                ps, pk = self.psum()
                psv = ps[:, :].rearrange("p (j t) -> p j t", j=4)
                fm_group(ps, pk, [(psv[:, j - g0, :], DQ + 128 * j, 128) for j in js])
                P.op('act', ACP(pad[ps_i][:, js[0]:js[-1] + 1, 2:130], psv[:, js[0] - g0:js[-1] - g0 + 1, :]),
                     reads=[pk], writes=[('pad', ps_i, g0)])
            if self.stop_after == 'projC':
                return
            if own:
                ps, pk = self.psum()
                for kc in range(8):
                    P.op('pe', MM(ps[:, :], hT[s][:, kc, :], w_in[:, kc, ZC:ZC + 512], kc == 0, kc == 7),
                         reads=[hk] + self.WINK, writes=[pk])
                zi = idx % 2
                P.op('act', ACT(zsb[zi][:], ps[:, :], AF.Silu), reads=[pk], writes=['zsb%d' % zi])
                P.op('pool', TT(zsb[zi][:], zsb[zi][:], self.wnbc[:].rearrange("p a b -> p (a b)"), ALU.mult),
                     reads=['zsb%d' % zi, 'wnbc'], writes=['zsb%d' % zi])
                P.op('sp', DMA(S['zs'][idx * 128:(idx + 1) * 128, :], zsb[zi][:]), reads=['zsb%d' % zi], dma='zsb%d' % zi)
            ps, pk = self.psum()
            for kc in range(8):
                P.op('pe', MM(ps[:, 0:144], hT[s][:, kc, :], w_in[:, kc, VA:VA + 144], kc == 0, kc == 7),
                     reads=[hk] + self.WINK, writes=[pk])
            if need_akv:
                if kind == 'ctx':
                    P.op('act', ACP(vc[:, idx, :, :], ps[:, 0:128].rearrange("p (h d) -> p h d", h=2)), reads=[pk], writes=[('vc', idx), 'vcopy'])
                else:
                    P.op('act', ACP(vr[idx % 4][:, :, :], ps[:, 0:128].rearrange("p (h d) -> p h d", h=2)), reads=[pk], writes=['vr%d' % (idx % 4), 'vcopy'])
            if self.stop_after == 'projD':
                return
            gi = cnt['x'] % 2
            g0tok = (idx * 128) if kind == 'ctx' else (256 + idx * 128)
            P.op('act', ACP(gbs[:], ps[:, 128:144]), reads=[pk, 'vcopy'], writes=['gbs'])
            P.op('dve', TS(gt[:, 0:8], gbs[:, 0:8], -1.0, ALU.mult), reads=['gbs'], writes=['gt0'])
            P.op('dve', TT(gt[:, 8:16], gbs[:, 8:16], self.dtb, ALU.add), reads=['gbs', 'adt'], writes=['gt1'])
            P.op('act', ACT(gl[:], gt[:], AF.Exp), reads=['gt0', 'gt1'], writes=['gl'])
            P.op('act', ACT(gl[:], gl[:], AF.Ln, bias=self.epsc[:, 2:3]), reads=['gl', 'epsc2'], writes=['gl'])
            P.op('dve', TT(gst[gi][:, 0:8], gl[:, 8:16], self.nexp[:], ALU.mult), reads=['gl', 'nexp'], writes=[('gst', gi, 0)])
            P.op('act', ACT(gst[gi][:, 8:16], gl[:, 0:8], AF.Exp, scale=-1.0), reads=['gl'], writes=[('gst', gi, 1)])
            P.op('dve', TS(gst[gi][:, 16:24], gl[:, 0:8], -1.0, ALU.mult), reads=['gl'], writes=[('gst', gi, 2)])
            if self.stop_after != 'projE':
              P.op('sp', DMA(S['gate'][g0tok:g0tok + 128, :], gst[gi][:]), reads=[('gst', gi, k) for k in range(3)],
                   writes=[('gate_s', g0tok)], dma='gst%d' % gi)

        def post(kind, idx, si):
            own = kind == 'loc' and idx < NT
            ps_i = si % 3
            g0tok = (idx * 128) if kind == 'ctx' else (256 + idx * 128)
            c = cnt['cs'] % 2
            cnt['cs'] += 1
            chunks = list(range(12)) if own else list(range(4, 12))
            padk = [('pad', ps_i, g0) for g0 in (0, 4, 8)] + [('padL', ps_i), ('padR', ps_i)]
            for j in chunks:
                P.op('dve', TS(cs[c][:, j, :], pad[ps_i][:, j, 0:128], self.cw[:, j, 0:1], ALU.mult),
                     reads=padk + ['cw'], writes=[('cs', c, j)])
                for k in range(1, 5):
                    P.op('dve', STT(cs[c][:, j, :], pad[ps_i][:, j, k:k + 128], self.cw[:, j, k:k + 1], cs[c][:, j, :], ALU.mult, ALU.add),
                         reads=padk + ['cw', ('cs', c, j)], writes=[('cs', c, j)])
            j0, j1 = chunks[0], chunks[-1] + 1
            csk = [('cs', c, j) for j in chunks]
            P.op('act', ACT(cs[c][:, j0:j1, :], cs[c][:, j0:j1, :], AF.Silu), reads=csk, writes=csk)
            qk0 = 0 if own else 4
            nq = 8 - qk0
            P.op('pool', TT(sq[:, qk0:8, :], cs[c][:, qk0:8, :], cs[c][:, qk0:8, :], ALU.mult), reads=csk, writes=['sq'])
            for g0 in range(qk0, 8, 4):
                ps, pk = self.psum()
                for j in range(g0, g0 + 4):
                    P.op('pe', MM(ps[:, (j - g0) * 128:(j - g0 + 1) * 128], self.ones_f, sq[:, j, :], True, True),
                         reads=['cf', 'sq'], writes=[pk])
                if g0 == 0:
                    P.op('act', ACT(rn[:, 0:4, :].rearrange("p a b -> p (a b)"), ps[:, :], AF.Ln, scale=128.0, bias=self.epsc[:, 1:2]),
                         reads=[pk, 'epsc1'], writes=[('rn', 0)])
                else:
                    P.op('act', ACT(rn[:, 4:8, :].rearrange("p a b -> p (a b)"), ps[:, :], AF.Ln, bias=self.epsc[:, 0:1]),
                         reads=[pk, 'epsc0'], writes=[('rn', 4)])
                P.op('act', ACT(rn[:, g0:g0 + 4, :], rn[:, g0:g0 + 4, :], AF.Exp, scale=-0.5), reads=[('rn', g0)], writes=[('rn', g0)])
                P.op('dve', TT(cs[c][:, g0:g0 + 4, :], cs[c][:, g0:g0 + 4, :], rn[:, g0:g0 + 4, :], ALU.mult),
                     reads=[('rn', g0)] + csk, writes=[('cs', c, j) for j in range(g0, g0 + 4)])
            P.op('sp', DMA(S['kT'][:, :, g0tok:g0tok + 128].rearrange("h d t -> d h t"), cs[c][:, 4:8, :]),
                 reads=[('cs', c, j) for j in range(4, 8)], writes=[('kT_s', g0tok)], dma='cs%d' % c)
            if own:
                P.op('sp', DMA(S['qT'][:, :, idx * 128:(idx + 1) * 128].rearrange("h d t -> d h t"), cs[c][:, 0:4, :]),
                     reads=[('cs', c, j) for j in range(0, 4)], writes=[('qT_s', idx)], dma='cs%d' % c)
            for (which, j0_, dst) in (('k', 4, S['ktok']), ('v', 8, S['vtok'])):
                ps, pk = self.psum()
                for j in range(4):
                    P.op('pe', TR(ps[:, j * 128:(j + 1) * 128], cs[c][:, j0_ + j, :], self.ident_f),
                         reads=[('cs', c, j0_ + j), 'cf'], writes=[pk])
                ti = cnt['tok'] % 2
                cnt['tok'] += 1
                P.op('act', ACP(tok[ti][:], ps[:, :]), reads=[pk], writes=['tok%d' % ti])
                P.op('sp', DMA(dst[g0tok:g0tok + 128, :], tok[ti][:]), reads=['tok%d' % ti], writes=[(which + 'tok_s', g0tok)], dma='tok%d' % ti)

        def attention(qb):
            qs = qTr[qb % 2]
            blocks = []
            if qb > 0:
                blocks.append(('L', kTr[(qb - 1) % 4], vr[(qb - 1) % 4], 'kTr%d' % ((qb - 1) % 4), 'vr%d' % ((qb - 1) % 4)))
            blocks.append(('C', kTr[qb % 4], vr[qb % 4], 'kTr%d' % (qb % 4), 'vr%d' % (qb % 4)))
            blocks.append(('R', kTr[(qb + 1) % 4], vr[(qb + 1) % 4], 'kTr%d' % ((qb + 1) % 4), 'vr%d' % ((qb + 1) % 4)))
            blocks.append(('X0', None, None, ('kTc', 0), ('vc', 0)))
            blocks.append(('X1', None, None, ('kTc', 1), ('vc', 1)))
            ai = cnt['att'] % 2
            cnt['att'] += 1
            qk = [('qTr', qb % 2, p) for p in range(4)]
            for kvh in range(2):
                pts = []
                for (typ, kt, vt, kk, vk) in blocks:
                    ps, pk = self.psum()
                    if typ[0] == 'X':
                        lhs = kTc[:, int(typ[1]), kvh, :]
                    else:
                        lhs = kt[:, kvh, :]
                    P.op('pe', MM(ps[:, :], lhs, qs[:, 4 * kvh:4 * kvh + 4, :].rearrange("p g q -> p (g q)"), True, True),
                         reads=[kk] + qk, writes=[pk])
                    pi = cnt['pT'] % 10
                    cnt['pT'] += 1
                    P.op('act', ACT(pT[pi][:], ps[:, :], AF.Exp, scale=0.125), reads=[pk], writes=['pT%d' % pi])
                    if typ in ('L', 'R'):
                        m = self.maskL if typ == 'L' else self.maskR
                        P.op('pool', TT(pT[pi][:].rearrange("p (g q) -> p g q", g=4), pT[pi][:].rearrange("p (g q) -> p g q", g=4),
                                        m.unsqueeze(1).to_broadcast([128, 4, 128]), ALU.mult),
                             reads=['pT%d' % pi, 'cb'], writes=['pT%d' % pi])
                    pts.append((typ, pi, vt, vk))
                pso, pko = self.psum()
                psz, pkz = self.psum()
                for bi, (typ, pi, vt, vk) in enumerate(pts):
                    if typ[0] == 'X':
                        lhs = vc[:, int(typ[1]), kvh, :]
                    else:
                        lhs = vt[:, kvh, :]
                    P.op('pe', MM(pso[0:64, :], lhs, pT[pi][:], bi == 0, bi == len(pts) - 1), reads=[vk, 'pT%d' % pi], writes=[pko])
                for bi, (typ, pi, vt, vk) in enumerate(pts):
                    P.op('pe', MM(psz[0:64, :], self.ones_b[:, 0:64], pT[pi][:], bi == 0, bi == len(pts) - 1), reads=['cb', 'pT%d' % pi], writes=[pkz])
                P.op('dve', TT(zt[:], psz[0:64, :], self.esink[:, 4 * kvh:4 * kvh + 4, :].rearrange("p g q -> p (g q)"), ALU.add),
                     reads=[pkz, 'esink'], writes=['zt'])
                P.op('dve', RECIP(zt[:], zt[:]), reads=['zt'], writes=['zt'])
                P.op('dve', TT(attT[ai][:, 4 * kvh:4 * kvh + 4, :].rearrange("p g q -> p (g q)"), pso[0:64, :], zt[:], ALU.mult),
                     reads=[pko, 'zt'], writes=[('attT', ai, kvh)])
            P.op('sp', DMA(S['attnT'][:, :, qb * 128:(qb + 1) * 128], attT[ai][:]), reads=[('attT', ai, 0), ('attT', ai, 1)],
                 writes=[('attnT_s', qb)], dma='attT%d' % ai)

        def run_seq(kind, tiles):
            for si, idx in enumerate(tiles):
                project(kind, idx, si)
                p = si % 3
                if si == 0:
                    P.op('pool', MEMSET(pad[p][:, :, 0:2], 0.0), writes=[('padL', p)])
                else:
                    pp = (si - 1) % 3
                    pk_prev = [('pad', pp, g0) for g0 in (0, 4, 8)]
                    pk_cur = [('pad', p, g0) for g0 in (0, 4, 8)]
                    P.op('pool', CP(pad[p][:, :, 0:2], pad[pp][:, :, 128:130]), reads=pk_prev, writes=[('padL', p)])
                    P.op('pool', CP(pad[pp][:, :, 130:132], pad[p][:, :, 2:4]), reads=pk_cur, writes=[('padR', pp)])
                    if self.stop_after not in ('proj', 'projA', 'projB', 'projC', 'projD', 'projE'):
                        post(kind, tiles[si - 1], si - 1)
                    if kind == 'loc' and 1 <= idx <= NT and self.stop_after not in ('proj', 'post', 'projA', 'projB', 'projC', 'projD', 'projE'):
                        attention(idx - 1)
            p = (len(tiles) - 1) % 3
            P.op('pool', MEMSET(pad[p][:, :, 130:132], 0.0), writes=[('padR', p)])
            if self.stop_after not in ('proj', 'projA', 'projB', 'projC', 'projD', 'projE'):
                post(kind, tiles[-1], len(tiles) - 1)

        for p in range(3):
            P.op('pool', MEMSET(pad[p][:], 0.0), writes=[('pad', p, g0) for g0 in (0, 4, 8)] + [('padL', p), ('padR', p)])
        run_seq('ctx', [0, 1])
        run_seq('loc', list(range(NL)))
        P.barrier()
        es.close()
        self.esA.close()


    def t_steps(self, es):
        sb = lambda name, shape, dt=F32: self.sb('t_' + name, shape, dt, es=es)
        tu = [sb('tu%d' % i, [128, 2, D]) for i in range(2)]
        tv = [sb('tv%d' % i, [128, 2, D]) for i in range(2)]
        to = [sb('to%d' % i, [128, 2, 2 * D], BF16) for i in range(2)]
        return self._t_gen(tu, tv, to)

    def _t_gen(self, tu, tv, to):
        P, I, S = self.P, self.I, self.S

        def load(st):
            b = st % 2
            r0 = st * 256
            P.op('sp', DMA(tu[b][:], I['peer_u'][r0:r0 + 256, :].rearrange("(p j) d -> p j d", j=2)), writes=['tu%d' % b], dma='t_u%d' % b)
            P.op('sp', DMA(tv[b][:], I['peer_v'][r0:r0 + 256, :].rearrange("(p j) d -> p j d", j=2)), writes=['tv%d' % b], dma='t_v%d' % b)
        load(0)
        for st in range(64):
            b = st % 2
            r0 = st * 256
            if st + 1 < 64:
                load(st + 1)
            P.op('act', ACP(to[b][:, :, 0:D], tu[b][:]), reads=['tu%d' % b], writes=[('to', b, 0)])
            P.op('dve', CP(to[b][:, :, D:2 * D], tv[b][:]), reads=['tv%d' % b], writes=[('to', b, 1)])
            P.op('pool', DMA(S['uv'][r0:r0 + 256, :].rearrange("(p j) d -> p j d", j=2), to[b][:]), reads=[('to', b, 0), ('to', b, 1)], dma='t_o%d' % b)
            yield

    def layer_norm_tile(self, pre, src, srck, dst, dstk, st6, mv, rstd):
        P = self.P
        for hh in range(2):
            P.op('dve', (lambda hh: lambda e: e.bn_stats(out=st6[:, hh, :], in_=src[:, hh * 512:(hh + 1) * 512]))(hh),
                 reads=srck, writes=[(pre + 'st6', hh)])
        P.op('dve', lambda e: e.bn_aggr(out=mv[:], in_=st6[:].rearrange("p a b -> p (a b)")), reads=[(pre + 'st6', 0), (pre + 'st6', 1)], writes=[pre + 'mv'])
        P.op('act', ACT(rstd[:], mv[:, 1:2], AF.Ln, bias=self.epsc[:, 0:1]), reads=[pre + 'mv'], writes=[pre + 'rstd'])
        P.op('act', ACT(rstd[:], rstd[:], AF.Exp, scale=-0.5), reads=[pre + 'rstd'], writes=[pre + 'rstd'])
        P.op('dve', TS(dst[:], src[:], mv[:, 0:1], ALU.subtract, rstd[:, 0:1], ALU.mult), reads=srck + [pre + 'mv', pre + 'rstd'], writes=dstk)

    def phaseM(self):
        P, I, S, NT = self.P, self.I, self.S, self.NT
        es = ExitStack()
        sb = lambda name, shape, dt=F32: self.sb('m_' + name, shape, dt, es=es)
        self.lnbc = sb('lnbc', [128, 4, D])
        P.op('sp', DMA(self.lnbc[:].rearrange("p a b -> p (a b)"), I['lnp'][:, :]), writes=['lnbc'], dma='*')
        wo_a = sb('wo_a', [64, 8, D], BF16)
        wo_g = sb('wo_g', [128, 4, D], BF16)
        for h in range(8):
            P.op('pool', DMA(wo_a[:, h, :], I['w_out'][h * 64:(h + 1) * 64, :]), writes=[('wo_a', h)], dma='m_wo')
        for c in range(4):
            P.op('pool', DMA(wo_g[:, c, :], I['w_out'][512 + c * 128:512 + (c + 1) * 128, :]), writes=[('wo_g', c)], dma='m_wo')
        WOK = [('wo_a', h) for h in range(8)] + [('wo_g', c) for c in range(4)]
        oa = [sb('oa%d' % i, [128, 4, 128]) for i in range(2)]
        obt = [sb('ob%d' % i, [128, 4, 128]) for i in range(2)]
        zs = [sb('zs%d' % i, [128, 512]) for i in range(2)]
        at = [sb('at%d' % i, [64, 8, 128], BF16) for i in range(2)]
        xt = [sb('xt%d' % i, [128, D]) for i in range(2)]
        osq = sb('osq', [128, 4, 128])
        ms = sb('ms', [128, 4])
        gdn = sb('gdn', [128, 512], BF16)
        gT = sb('gT', [128, 4, 128], BF16)
        rr = sb('rr', [128, D])
        st6 = sb('st6', [128, 2, 6]); mv = sb('mv', [128, 2]); rstd = sb('rstd', [128, 1])
        x1 = [sb('x1%d' % i, [128, D]) for i in range(2)]
        f2 = lambda t: t[:].rearrange("p a b -> p (a b)")
        for idx in range(NT):
            b = idx % 2
            r0 = idx * 128
            P.op('sp', DMA(f2(oa[b]), S['oA'][r0:r0 + 128, :]), reads=[('ga_o_s', idx)], writes=['oa%d' % b], dma='m_oa%d' % b)
            P.op('sp', DMA(f2(obt[b]), S['oB'][r0:r0 + 128, :]), reads=[('gb_o_s', idx)], writes=['ob%d' % b], dma='m_ob%d' % b)
            P.op('sp', DMA(zs[b][:], S['zs'][r0:r0 + 128, :]), writes=['zs%d' % b], dma='m_zs%d' % b)
            P.op('sp', DMA(at[b][:], S['attnT'][:, :, r0:r0 + 128]), writes=['at%d' % b], dma='m_at%d' % b)
            P.op('sp', DMA(xt[b][:], I['x'][r0:r0 + 128, :]), writes=['xt%d' % b], dma='m_xt%d' % b)
            P.op('pool', TT(f2(oa[b]), f2(oa[b]), f2(obt[b]), ALU.add), reads=['oa%d' % b, 'ob%d' % b], writes=['oa%d' % b])
            P.op('pool', TT(f2(osq), f2(oa[b]), f2(oa[b]), ALU.mult), reads=['oa%d' % b], writes=['osq'])
            P.op('dve', lambda e, b=b: e.tensor_reduce(out=ms[:], in_=osq[:], axis=AX.X, op=ALU.add), reads=['osq'], writes=['ms'])
            P.op('act', ACT(ms[:], ms[:], AF.Ln, scale=1.0 / 128.0, bias=self.epsc[:, 0:1]), reads=['ms'], writes=['ms'])
            P.op('act', ACT(ms[:], ms[:], AF.Exp, scale=-0.5), reads=['ms'], writes=['ms'])
            P.op('dve', TT(oa[b][:], oa[b][:], ms[:, :].unsqueeze(2).to_broadcast([128, 4, 128]), ALU.mult), reads=['oa%d' % b, 'ms'], writes=['oa%d' % b])
            P.op('pool', TT(gdn[:], f2(oa[b]), zs[b][:], ALU.mult), reads=['oa%d' % b, 'zs%d' % b], writes=['gdn'])
            for c in range(4):
                P.op('pe', TR(self.psb[:, c * 128:(c + 1) * 128], gdn[:, c * 128:(c + 1) * 128], self.ident_b), reads=['gdn', 'cb'], writes=['psb'])
            P.op('act', ACP(f2(gT), self.psb[:, 0:512]), reads=['psb'], writes=['gT'])
            for nh in range(2):
                ps, pk = self.psum()
                ns = slice(nh * 512, (nh + 1) * 512)
                for h in range(8):
                    P.op('pe', MM(ps[:, :], at[b][:, h, :], wo_a[:, h, ns], h == 0, False), reads=['at%d' % b] + WOK, writes=[pk])
                for c in range(4):
                    P.op('pe', MM(ps[:, :], gT[:, c, :], wo_g[:, c, ns], False, c == 3), reads=['gT'] + WOK, writes=[pk])
                P.op('dve', TT(rr[:, ns], ps[:, :], self.modb[:, 2 * D + nh * 512:2 * D + (nh + 1) * 512], ALU.mult), reads=[pk], writes=[('rr', nh)])
            P.op('dve', STT(rr[:], xt[b][:], ALPHA, rr[:], ALU.mult, ALU.add), reads=['xt%d' % b, ('rr', 0), ('rr', 1)], writes=[('rr', 0), ('rr', 1)])
            self.layer_norm_tile('m_', rr, [('rr', 0), ('rr', 1)], rr, [('rr', 0), ('rr', 1)], st6, mv, rstd)
            P.op('pool', TT(rr[:], rr[:], self.lnbc[:, 0, :], ALU.mult), reads=[('rr', 0), ('rr', 1), 'lnbc'], writes=[('rr', 0), ('rr', 1)])
            P.op('dve', TT(x1[b][:], rr[:], self.lnbc[:, 1, :], ALU.add), reads=[('rr', 0), ('rr', 1), 'lnbc'], writes=['x1%d' % b])
            P.op('pool', DMA(S['x1'][r0:r0 + 128, :], x1[b][:]), reads=['x1%d' % b], writes=[('x1_s', idx)], dma='m_x1%d' % b)
        P.barrier()
        es.close()

    def phaseC(self):
        P, I, S, NT = self.P, self.I, self.S, self.NT
        es = ExitStack()
        sb = lambda name, shape, dt=F32: self.sb('c_' + name, shape, dt, es=es)
        self.lnbc = sb('lnbc', [128, 4, D])
        P.op('sp', DMA(self.lnbc[:].rearrange("p a b -> p (a b)"), I['lnp'][:, :]), writes=['lnbc'], dma='*')
        wq = sb('wq', [128, 8, 2048], BF16)
        for kc in range(8):
            P.op('pool', DMA(wq[:, kc, :], I['wq'][kc * 128:(kc + 1) * 128, :]), writes=[('wq', kc)], dma='c_wq')
        WQK = [('wq', kc) for kc in range(8)]
        skT = sb('skT', [128, 2, 128])
        P.op('sp', DMA(skT[:], I['skT'][:, :, :]), writes=['skT'], dma='*')
        iota16 = self.cf[:, 832:848]
        x1t = [sb('x1t%d' % i, [128, D]) for i in range(2)]
        xn = sb('xn', [128, D])
        h2_2 = [sb('h2_%d' % i, [128, D]) for i in range(2)]
        h2b = sb('h2b', [128, D], BF16)
        h2T = sb('h2T', [128, 8, 128], BF16)
        st6 = sb('st6', [128, 2, 6]); mv = sb('mv', [128, 2]); rstd = sb('rstd', [128, 1])
        st6b = sb('st6b', [128, 2, 6]); mvb = sb('mvb', [128, 2]); rstdb = sb('rstdb', [128, 1])
        sc = sb('sc', [128, 16, 128]); sc2 = sb('sc2', [128, 16, 128])
        qTs = sc2
        stop_ = sb('stop', [128, 16, 16]); itop = sb('itop', [128, 16, 16], U32); itf = sb('itf', [128, 16, 16])
        cand = sb('cand', [128, 8, 256])
        cand2 = sc[:].rearrange("p a b -> p (a b)").rearrange("p (h c) -> p h c", h=8)
        best = sb('best', [128, 8, 16]); pos = sb('pos', [128, 8, 16], U32)
        pa = sb('pa', [128, 8, 16], I32); pb = sb('pb', [128, 8, 16], I32)
        paf = sb('paf', [128, 8, 16]); pbf = sb('pbf', [128, 8, 16])
        eq = sb('eq', [128, 8, 16, 16], BF16)
        i1 = sb('i1', [128, 8, 16]); i2 = sb('i2', [128, 8, 16])
        eidf = sb('eidf', [128, 128]); eidx_2 = [sb('eidx%d' % i, [128, 128], I32) for i in range(2)]
        ge_2 = [sb('ge%d' % i, [128, 8, 16]) for i in range(2)]; gsum = sb('gsum', [128, 8])
        apre = sb('apre', [128, 128]); wgt = sb('wgt', [128, 128])
        junk = sb('junk', [128, D], BF16)
        yacc = sb('yacc', [128, D])
        ot = [sb('ot0', [128, D])] * 2
        wg4 = sb('wg4', [128, 128])
        dg = [sb('dg%d' % i, [128, 128], BF16) for i in range(8)]
        NR = int(min(16, self.nc.sbuf_bytes_remaining // 4096 - 1))
        assert NR >= 8, NR
        ring = [sb('ring%d' % i, [128, 2 * D], BF16) for i in range(NR)]
        f2 = lambda t: t[:].rearrange("p a b -> p (a b)")
        rcnt = 0
        self.ps_lim = 5
        self.ps_next = 0
        def front_gen(idx):
            b = idx % 2
            r0 = idx * 128
            h2, eidx, ge = h2_2[b], eidx_2[b], ge_2[b]
            kh2, keidx, kge = 'h2_%d' % b, 'eidx%d' % b, 'ge%d' % b
            P.op('sp', DMA(x1t[b][:], S['x1'][r0:r0 + 128, :]), reads=[('x1_s', idx)], writes=['x1t%d' % b], dma='c_x1t%d' % b)
            self.layer_norm_tile('c_', x1t[b], ['x1t%d' % b], xn, ['xn'], st6, mv, rstd)
            P.op('pool', TT(xn[:], xn[:], self.modb[:, 4 * D:5 * D], ALU.mult), reads=['xn'], writes=['xn'])
            P.op('dve', TT(h2[:], xn[:], self.modb[:, 3 * D:4 * D], ALU.add), reads=['xn'], writes=[kh2])
            P.op('act', ACP(h2b[:], h2[:]), reads=[kh2], writes=['h2b'])
            yield
            for kc in range(8):
                P.op('pe', TR(self.psb[:, kc * 128:(kc + 1) * 128], h2b[:, kc * 128:(kc + 1) * 128], self.ident_b), reads=['h2b', 'cb'], writes=['psb'])
            P.op('act', ACP(f2(h2T), self.psb[:, :]), reads=['psb'], writes=['h2T'])
            yield
            for g0 in range(0, 16, 4):
                ps, pk = self.psum()
                for hx in range(g0, g0 + 4):
                    for kc in range(8):
                        P.op('pe', MM(ps[:, (hx - g0) * 128:(hx - g0 + 1) * 128], wq[:, kc, hx * 128:(hx + 1) * 128], h2T[:, kc, :], kc == 0, kc == 7),
                             reads=['h2T'] + WQK, writes=[pk])
                P.op('act', ACP(qTs[:, g0:g0 + 4, :].rearrange("p a b -> p (a b)"), ps[:, :]), reads=[pk], writes=[('qTs', g0), 'sc2all'])
                yield
            for g0 in range(0, 16, 4):
                ps, pk = self.psum()
                for hx in range(g0, g0 + 4):
                    P.op('pe', MM(ps[:, (hx - g0) * 128:(hx - g0 + 1) * 128], qTs[:, hx, :], skT[:, hx % 2, :], True, True),
                         reads=[('qTs', g0), 'skT', 'sc2all'], writes=[pk])
                P.op('act', ACP(sc[:, g0:g0 + 4, :].rearrange("p a b -> p (a b)"), ps[:, :]), reads=[pk], writes=[('sc', g0), 'scall'])
                yield
            yield
            for hx in range(16):
                sk_ = ('sc', 4 * (hx // 4))
                P.op('dve', (lambda hx: lambda e: e.max(out=stop_[:, hx, 0:8], in_=sc[:, hx, :]))(hx), reads=[sk_, 'scall'], writes=[('stop', hx, 0)])
                P.op('dve', (lambda hx: lambda e: e.match_replace(out=sc2[:, hx, :], in_to_replace=stop_[:, hx, 0:8], in_values=sc[:, hx, :], imm_value=-1e30))(hx),
                     reads=[sk_, 'scall', ('stop', hx, 0)], writes=[('sc2', hx), 'sc2all'])
                P.op('dve', (lambda hx: lambda e: e.max(out=stop_[:, hx, 8:16], in_=sc2[:, hx, :]))(hx), reads=[('sc2', hx), 'sc2all'], writes=[('stop', hx, 1)])
                P.op('dve', (lambda hx: lambda e: e.max_index(out=itop[:, hx, 0:8], in_max=stop_[:, hx, 0:8], in_values=sc[:, hx, :]))(hx),
                     reads=[sk_, 'scall', ('stop', hx, 0)], writes=[('itop', hx, 0)])
                P.op('dve', (lambda hx: lambda e: e.max_index(out=itop[:, hx, 8:16], in_max=stop_[:, hx, 8:16], in_values=sc2[:, hx, :]))(hx),
                     reads=[('sc2', hx), 'sc2all', ('stop', hx, 1)], writes=[('itop', hx, 1)])
                if hx % 2 == 1:
                    yield
            yield
            STK = [('stop', hx, k) for hx in range(16) for k in range(2)]
            ITK = [('itop', hx, k) for hx in range(16) for k in range(2)]
            P.op('dve', CP(f2(itf), f2(itop)), reads=ITK, writes=['itf'])
            sv = stop_[:].rearrange("p (h x) a -> p h x a", x=2)
            iv = itf[:].rearrange("p (h x) a -> p h x a", x=2)
            c4 = cand[:].rearrange("p h (a b) -> p h a b", a=16)
            P.op('dve', TT(c4, sv[:, :, 0, :].unsqueeze(3).to_broadcast([128, 8, 16, 16]), sv[:, :, 1, :].unsqueeze(2).to_broadcast([128, 8, 16, 16]), ALU.add),
                 reads=STK, writes=['cand'])
            for h in range(8):
                P.op('dve', (lambda h: lambda e: e.max(out=best[:, h, 0:8], in_=cand[:, h, :]))(h), reads=['cand'], writes=[('best', h, 0)])
                P.op('dve', (lambda h: lambda e: e.match_replace(out=cand2[:, h, :], in_to_replace=best[:, h, 0:8], in_values=cand[:, h, :], imm_value=-1e30))(h),
                     reads=['cand', ('best', h, 0)], writes=[('cand2', h), 'scall'])
                P.op('dve', (lambda h: lambda e: e.max(out=best[:, h, 8:16], in_=cand2[:, h, :]))(h), reads=[('cand2', h), 'scall'], writes=[('best', h, 1)])
                P.op('dve', (lambda h: lambda e: e.max_index(out=pos[:, h, 0:8], in_max=best[:, h, 0:8], in_values=cand[:, h, :]))(h),
                     reads=['cand', ('best', h, 0)], writes=[('pos', h, 0)])
                P.op('dve', (lambda h: lambda e: e.max_index(out=pos[:, h, 8:16], in_max=best[:, h, 8:16], in_values=cand2[:, h, :]))(h),
                     reads=[('cand2', h), 'scall', ('best', h, 1)], writes=[('pos', h, 1)])
                if h % 2 == 1:
                    yield
            yield
            BK = [('best', h, k) for h in range(8) for k in range(2)]
            PK = [('pos', h, k) for h in range(8) for k in range(2)]
            posi = pos[:].bitcast(I32)
            P.op('dve', lambda e: e.tensor_single_scalar(out=f2(pa), in_=posi.rearrange("p a b -> p (a b)"), scalar=4, op=ALU.arith_shift_right), reads=PK, writes=['pa'])
            P.op('dve', lambda e: e.tensor_single_scalar(out=f2(pb), in_=posi.rearrange("p a b -> p (a b)"), scalar=15, op=ALU.bitwise_and), reads=PK, writes=['pb'])
            P.op('dve', CP(f2(paf), f2(pa)), reads=['pa'], writes=['paf'])
            P.op('dve', CP(f2(pbf), f2(pb)), reads=['pb'], writes=['pbf'])
            yield
            io4 = iota16.unsqueeze(1).unsqueeze(1).to_broadcast([128, 8, 16, 16])
            for (pf, pfk, x_, dst, dk_) in ((paf, 'paf', 0, i1, 'i1'), (pbf, 'pbf', 1, i2, 'i2')):
                P.op('dve', TT(eq[:], io4, pf[:, :, :].unsqueeze(3).to_broadcast([128, 8, 16, 16]), ALU.is_equal), reads=['cf', pfk], writes=['eq'])
                P.op('dve', TT(eq[:], eq[:], iv[:, :, x_, :].unsqueeze(2).to_broadcast([128, 8, 16, 16]), ALU.mult), reads=['eq', 'itf'], writes=['eq'])
                P.op('dve', (lambda dst: lambda e: e.tensor_reduce(out=dst[:], in_=eq[:], axis=AX.X, op=ALU.add))(dst), reads=['eq'], writes=[dk_])
            P.op('dve', STT(eidf[:], f2(i1), 128.0, f2(i2), ALU.mult, ALU.add), reads=['i1', 'i2'], writes=['eidf'])
            P.op('dve', CP(eidx[:], eidf[:]), reads=['eidf'], writes=[keidx])
            yield
            P.op('dve', TT(ge[:], best[:], best[:, :, 0:1].to_broadcast([128, 8, 16]), ALU.subtract), reads=BK, writes=[kge])
            P.op('act', ACT(f2(ge), f2(ge), AF.Exp), reads=[kge], writes=[kge])
            P.op('dve', lambda e: e.tensor_reduce(out=gsum[:], in_=ge[:], axis=AX.X, op=ALU.add), reads=[kge], writes=['gsum'])
            P.op('dve', RECIP(gsum[:], gsum[:]), reads=['gsum'], writes=['gsum'])
            P.op('dve', TT(ge[:], ge[:], gsum[:, :].unsqueeze(2).to_broadcast([128, 8, 16]), ALU.mult), reads=[kge, 'gsum'], writes=[kge])
            yield

        def loop_gen(idx):
            nonlocal rcnt
            b = idx % 2
            r0 = idx * 128
            h2, eidx, ge = h2_2[b], eidx_2[b], ge_2[b]
            kh2, keidx, kge = 'h2_%d' % b, 'eidx%d' % b, 'ge%d' % b
            psY = [(self.psf[5], 'ps5'), (self.psf[6], 'ps6')]
            GS = 4
            dcnt = 0
            for g in range(0, 128, GS):
                ris = []
                for slot in range(g, g + GS):
                    ri = rcnt % NR
                    rcnt += 1
                    ris.append(ri)
                    P.op('pool', (lambda ri, slot: lambda e: e.indirect_dma_start(out=ring[ri][:], out_offset=None, in_=S['uv'][:, :],
                         in_offset=bass.IndirectOffsetOnAxis(ap=eidx[:, slot:slot + 1], axis=0)))(ri, slot), reads=[keidx], writes=['ring%d' % ri], dma='c_ring%d' % ri)
                    P.op('dve', STT(junk[:], ring[ri][:, 0:D], 1.0, h2[:], ALU.mult, ALU.mult, accum=apre[:, slot:slot + 1]), reads=['ring%d' % ri, kh2], writes=[('apre', slot), 'junk'])
                AKg = [('apre', sl) for sl in range(g, g + GS)]
                P.op('act', ACT(wg4[:, g:g + GS], apre[:, g:g + GS], AF.Gelu), reads=AKg, writes=[('wg4', g)])
                P.op('dve', TT(wgt[:, g:g + GS], wg4[:, g:g + GS], f2(ge)[:, g:g + GS], ALU.mult), reads=[('wg4', g), kge], writes=[('wgt', g)])
                for k, slot in enumerate(range(g, g + GS)):
                    di = dcnt % 8
                    dcnt += 1
                    ri = ris[k]
                    P.op('act', ACT(dg[di][:], self.ident_b, AF.Copy, scale=wgt[:, slot:slot + 1]), reads=['cb', ('wgt', g)], writes=['dg%d' % di])
                    for nh in range(2):
                        P.op('pe', MM(psY[nh][0][:, :], dg[di][:], ring[ri][:, D + nh * 512:D + (nh + 1) * 512], slot == 0, slot == 127),
                             reads=['dg%d' % di, 'ring%d' % ri], writes=[psY[nh][1]])
                yield
            yield
            for nh in range(2):
                P.op('dve', TT(yacc[:, nh * 512:(nh + 1) * 512], psY[nh][0][:, :], self.modb[:, 5 * D + nh * 512:5 * D + (nh + 1) * 512], ALU.mult),
                     reads=[psY[nh][1]], writes=[('yacc', nh)])
            P.op('dve', STT(yacc[:], x1t[b][:], ALPHA, yacc[:], ALU.mult, ALU.add), reads=['x1t%d' % b, ('yacc', 0), ('yacc', 1)], writes=['yacc', ('yacc', 0), ('yacc', 1)])
            self.layer_norm_tile('c2_', yacc, ['yacc'], yacc, ['yacc'], st6b, mvb, rstdb)
            P.op('pool', TT(yacc[:], yacc[:], self.lnbc[:, 2, :], ALU.mult), reads=['yacc', 'lnbc'], writes=['yacc'])
            P.op('dve', TT(ot[b][:], yacc[:], self.lnbc[:, 3, :], ALU.add), reads=['yacc', 'lnbc'], writes=['ot0'])
            P.op('sp', DMA(self.out[r0:r0 + 128, :], ot[b][:]), reads=['ot0'], dma='c_ot%d' % b)
            yield

        def drain(*gens):
            gens = [x for x in gens if x is not None]
            while gens:
                for x in list(gens):
                    try:
                        next(x)
                    except StopIteration:
                        gens.remove(x)
        prev = None
        for idx in range(NT):
            drain(front_gen(idx), prev)
            prev = loop_gen(idx)
        drain(prev)
        self.ps_lim = 7
        P.barrier()
        es.close()

    def gdn_pass(self, tag, base, go, tiles, out_ap, extra=None):
        P, S = self.P, self.S
        es = ExitStack()
        sb = lambda name, shape, dt=F32: self.sb(tag + name, shape, dt, es=es)
        cf = self.cf
        U = cf[0:64, base:base + 64]
        mbiT = cf[0:64, base + 64:base + 128]
        mbsT = cf[0:64, base + 128:base + 192]
        mbs = cf[0:64, base + 192:base + 256]
        I64 = cf[0:64, 768:832]
        ones64 = cf[0:64, 128:192]
        ones64x128 = cf[0:64, 128:256]
        kTt = [sb('kTt%d' % i, [128, 4, 128]) for i in range(2)]
        qTt = [sb('qTt%d' % i, [128, 4, 128]) for i in range(2)]
        ktk = [sb('ktk%d' % i, [64, 2, 4, 128]) for i in range(2)]
        vtk = [sb('vtk%d' % i, [64, 2, 4, 128]) for i in range(2)]
        gts = [sb('gts%d' % i, [64, 2, 24]) for i in range(2)]
        gsb = sb('gsb', [128, 16])
        egam = sb('egam', [64, 8])
        ekd = sb('ekd', [64, 8])
        egt2 = [sb('egt%d' % i, [128, 8]) for i in range(2)]
        bege = sb('bege', [64, 8])
        gpl = sb('gpl', [64, 8])
        G1 = sb('G1', [64, 8, 64]); G1n = sb('G1n', [64, 8, 64]); G1b = sb('G1b', [64, 8, 64])
        MaT = sb('MaT', [64, 8, 64]); Ma = sb('Ma', [64, 8, 64]); Dq = sb('Dq', [64, 8, 64])
        egb = sb('egb', [128, 8, 64])
        DTX = F32
        XY = [[sb('X%d' % i, [64, 8, 64], DTX), sb('Y%d' % i, [64, 8, 64], DTX)] for i in range(2)]
        R = sb('R', [64, 8, 64], DTX)
        QKm2 = [sb('QKm%d' % i, [64, 8, 64], BF16) for i in range(2)]
        vb = sb('vb', [64, 8, 128], DTX); kbg = sb('kbg', [64, 8, 128], DTX); Kd2 = [sb('Kd%d' % i, [64, 8, 128], BF16) for i in range(2)]
        usb2 = [sb('usb%d' % i, [64, 8, 128]) for i in range(2)]; WT2 = [sb('WT%d' % i, [128, 8, 64]) for i in range(2)]; QdT2 = [sb('QdT%d' % i, [128, 8, 64]) for i in range(2)]
        vnew = sb('vnew', [64, 4, 128], BF16)
        Sst = [sb('Sst%d' % i, [128, 4, 128]) for i in range(2)]
        Sb = sb('Sb', [128, 4, 128], BF16)
        kTb = sb('kTb', [128, 4, 128], BF16); qTb = sb('qTb', [128, 4, 128], BF16)
        I64b = I64
        osb = [sb('osb%d' % i, [64, 2, 4, 128]) for i in range(2)]
        P.op('pool', MEMSET(Sst[0][:], 0.0), writes=['Sst0'])
        P.op('pool', MEMSET(Sb[:], 0.0), writes=['Sb'])
        cur = 0
        rev = base == 512
        f2 = lambda t: t[:].rearrange("p a b -> p (a b)")
        def prep_gen(ti, g0, oi):
            b = ti % 2
            egt, QKm, Kd, usb, WT, QdT = egt2[b], QKm2[b], Kd2[b], usb2[b], WT2[b], QdT2[b]
            kegt, kQKm, kKd, kusb, kWT, kQdT = 'egt%d' % b, 'QKm%d' % b, 'Kd%d' % b, 'usb%d' % b, 'WT%d' % b, 'QdT%d' % b
            want_o = oi is not None
            P.op('sp', DMA(kTt[b][:], S['kT'][:, :, g0:g0 + 128].rearrange("h d t -> d h t")), reads=[('kT_s', g0)], writes=['kTt%d' % b], dma=tag + 'kTt%d' % b)
            P.op('sp', DMA(ktk[b][:].rearrange("p c h e -> p c (h e)"), S['ktok'][g0:g0 + 128, :].rearrange("(c p) f -> p c f", p=64)),
                 reads=[('ktok_s', g0)], writes=['ktk%d' % b], dma=tag + 'ktk%d' % b)
            P.op('sp', DMA(vtk[b][:].rearrange("p c h e -> p c (h e)"), S['vtok'][g0:g0 + 128, :].rearrange("(c p) f -> p c f", p=64)),
                 reads=[('vtok_s', g0)], writes=['vtk%d' % b], dma=tag + 'vtk%d' % b)
            P.op('sp', DMA(gts[b][:], S['gate'][g0:g0 + 128, :].rearrange("(c p) f -> p c f", p=64)),
                 reads=[('gate_s', g0)], writes=['gts%d' % b], dma=tag + 'gts%d' % b)
            if want_o:
                P.op('sp', DMA(qTt[b][:], S['qT'][:, :, oi * 128:(oi + 1) * 128].rearrange("h d t -> d h t")), reads=[('qT_s', oi)], writes=['qTt%d' % b], dma=tag + 'qTt%d' % b)
            P.op('pool', CP(kTb[:], kTt[b][:]), reads=['kTt%d' % b], writes=['kTb'])
            if want_o:
                P.op('pool', CP(qTb[:], qTt[b][:]), reads=['qTt%d' % b], writes=['qTb'])
            gk = 'gts%d' % b
            gg = gts[b][:, :, go:go + 4]
            gbeta = gts[b][:, :, 8 + go:12 + go]
            glnb = gts[b][:, :, 16 + go:20 + go]
            ps, pk = self.psum()
            for c in range(2):
                P.op('pe', MM(ps[0:64, 4 * c:4 * c + 4], U, gts[b][:, c, go:go + 4], True, True), reads=['cf', gk], writes=[pk])
                P.op('pe', MM(ps[:, 8 + 4 * c:12 + 4 * c], ones64x128, gts[b][:, c, go:go + 4], True, True), reads=['cf', gk], writes=[pk])
            P.op('act', ACP(gsb[0:64, 0:8], ps[0:64, 0:8]), reads=[pk], writes=['gsb0'])
            P.op('act', ACP(gsb[:, 8:16], ps[:, 8:16]), reads=[pk], writes=['gsb1'])
            P.op('act', ACT(egam[:], gsb[0:64, 0:8], AF.Exp), reads=['gsb0'], writes=['egam'])
            P.op('dve', TT(ekd[:], gsb[0:64, 8:16], gsb[0:64, 0:8], ALU.subtract), reads=['gsb0', 'gsb1'], writes=['ekd'])
            P.op('act', ACT(ekd[:], ekd[:], AF.Exp), reads=['ekd'], writes=['ekd'])
            P.op('act', ACT(egt[:], gsb[:, 8:16], AF.Exp), reads=['gsb1'], writes=[kegt])
            yield
            v4 = lambda t: t[:].rearrange("p (c h) i -> p c h i", c=2)
            Ub = U.unsqueeze(1).unsqueeze(1).to_broadcast([64, 2, 4, 64])
            Ib = I64.unsqueeze(1).unsqueeze(1).to_broadcast([64, 2, 4, 64])
            P.op('dve', TT(v4(G1), Ub, gg.unsqueeze(3).to_broadcast([64, 2, 4, 64]), ALU.mult), reads=['cf', gk], writes=['G1'])
            P.op('pool', TS(f2(G1n), f2(G1), -1.0, ALU.mult), reads=['G1'], writes=['G1n'])
            P.op('pool', TT(v4(G1b), Ib, glnb.unsqueeze(3).to_broadcast([64, 2, 4, 64]), ALU.mult), reads=['cf', gk], writes=['G1b'])
            P.op('pool', TT(f2(G1b), f2(G1b), f2(G1), ALU.add), reads=['G1b', 'G1'], writes=['G1b'])
            yield
            psA, pkA = self.psum()
            psB, pkB = self.psum()
            P.op('pe', MM(psA[0:64, :], ones64, f2(G1b), True, True), reads=['cf', 'G1b'], writes=[pkA])
            P.op('pe', MM(psB[0:64, :], ones64, f2(G1n), True, True), reads=['cf', 'G1n'], writes=[pkB])
            m3 = lambda m: m.unsqueeze(1).to_broadcast([64, 8, 64])
            p3 = lambda p_: p_[0:64, :].rearrange("p (a b) -> p a b", a=8)
            gcol = gsb[0:64, 0:8].unsqueeze(2).to_broadcast([64, 8, 64])
            P.op('dve', TT(gpl[:].rearrange("p (c h) -> p c h", c=2), gsb[0:64, 0:8].rearrange("p (c h) -> p c h", c=2), glnb, ALU.add), reads=['gsb0', gk], writes=['gpl'])
            gplc = gpl[:, :].unsqueeze(2).to_broadcast([64, 8, 64])
            P.op('dve', TT(MaT[:], p3(psA), m3(mbsT), ALU.add), reads=[pkA, 'cf'], writes=['MaT'])
            P.op('dve', TT(MaT[:], MaT[:], gcol, ALU.subtract), reads=['MaT', 'gsb0'], writes=['MaT'])
            P.op('act', ACT(f2(MaT), f2(MaT), AF.Exp), reads=['MaT'], writes=['MaT'])
            P.op('dve', TT(Ma[:], p3(psB), m3(mbs), ALU.add), reads=[pkB, 'cf'], writes=['Ma'])
            P.op('dve', TT(Ma[:], Ma[:], gplc, ALU.add), reads=['Ma', 'gpl'], writes=['Ma'])
            P.op('act', ACT(f2(Ma), f2(Ma), AF.Exp), reads=['Ma'], writes=['Ma'])
            if want_o:
                psC, pkC = self.psum()
                psD, pkD = self.psum()
                P.op('pe', MM(psC[0:64, :], ones64, f2(G1), True, True), reads=['cf', 'G1'], writes=[pkC])
                P.op('pe', MM(psD[:, :], ones64x128, f2(G1), True, True), reads=['cf', 'G1'], writes=[pkD])
                P.op('dve', TT(Dq[:], p3(psC), m3(mbiT), ALU.add), reads=[pkC, 'cf'], writes=['Dq'])
                P.op('dve', TT(Dq[:], Dq[:], gcol, ALU.subtract), reads=['Dq', 'gsb0'], writes=['Dq'])
                P.op('act', ACT(f2(Dq), f2(Dq), AF.Exp), reads=['Dq'], writes=['Dq'])
                P.op('act', ACT(f2(egb), psD[:, :], AF.Exp), reads=[pkD], writes=['egb'])
            yield
            psK, pkK = self.psum()
            for it in range(8):
                c, h = it // 4, it % 4
                sl = slice(it * 64, (it + 1) * 64)
                kc_ = kTb[:, h, c * 64:(c + 1) * 64]
                P.op('pe', MM(psK[0:64, sl], kc_, kc_, True, True), reads=['kTb'], writes=[pkK])
            X0, Y0 = XY[0]
            P.op('dve', TT(f2(X0), psK[0:64, :], f2(MaT), ALU.mult), reads=[pkK, 'MaT'], writes=['X0'])
            P.op('dve', TT(f2(Y0), psK[0:64, :], f2(Ma), ALU.mult), reads=[pkK, 'Ma'], writes=['Y0'])
            if want_o:
                psQ, pkQ = self.psum()
                for it in range(8):
                    c, h = it // 4, it % 4
                    sl = slice(it * 64, (it + 1) * 64)
                    P.op('pe', MM(psQ[0:64, sl], kTb[:, h, c * 64:(c + 1) * 64], qTb[:, h, c * 64:(c + 1) * 64], True, True),
                         reads=['kTb', 'qTb'], writes=[pkQ])
                P.op('dve', TT(f2(QKm), psQ[0:64, :], f2(Dq), ALU.mult), reads=[pkQ, 'Dq'], writes=[kQKm])
                P.op('pool', TT(QdT[:].rearrange("p (c h) i -> p c h i", c=2), qTt[b][:].rearrange("p h (c i) -> p c h i", c=2),
                                egb[:].rearrange("p (c h) i -> p c h i", c=2), ALU.mult), reads=['qTt%d' % b, 'egb'], writes=[kQdT])
            yield
            P.op('pool', TT(R[:], m3(I64), X0[:], ALU.subtract), reads=['cf', 'X0'], writes=['R'])
            pp = 0
            for lvl in range(5):
                Xc, Yc = XY[pp]
                Xn, Yn = XY[1 - pp]
                kx, ky, nx, ny = 'X%d' % pp, 'Y%d' % pp, 'X%d' % (1 - pp), 'Y%d' % (1 - pp)
                psY, pkY = self.psum()
                for it in range(8):
                    sl = slice(it * 64, (it + 1) * 64)
                    P.op('pe', MM(psY[0:64, sl], Xc[:, it, :], Yc[:, it, :], True, True), reads=[kx, ky], writes=[pkY])
                P.op('act', ACP(f2(Yn), psY[0:64, :]), reads=[pkY], writes=[ny])
                if lvl < 4:
                    psX, pkX = self.psum()
                    for it in range(8):
                        sl = slice(it * 64, (it + 1) * 64)
                        P.op('pe', MM(psX[0:64, sl], Yc[:, it, :], Xc[:, it, :], True, True), reads=[kx, ky], writes=[pkX])
                    P.op('act', ACP(f2(Xn), psX[0:64, :]), reads=[pkX], writes=[nx])
                psR, pkR = self.psum()
                for it in range(8):
                    sl = slice(it * 64, (it + 1) * 64)
                    P.op('pe', MM(psR[0:64, sl], Yn[:, it, :], R[:, it, :], True, True), reads=[ny, 'R'], writes=[pkR])
                P.op('dve', TT(f2(R), f2(R), psR[0:64, :], ALU.add), reads=[pkR, 'R'], writes=['R'])
                pp = 1 - pp
                yield
            yield
            b4 = lambda a: a.unsqueeze(3).to_broadcast([64, 2, 4, 128])
            e4 = lambda t: t[:].rearrange("p (c h) e -> p c h e", c=2)
            P.op('dve', TT(bege[:].rearrange("p (c h) -> p c h", c=2), gbeta, egam[:].rearrange("p (c h) -> p c h", c=2), ALU.mult), reads=[gk, 'egam'], writes=['bege'])
            P.op('pool', TT(e4(vb), vtk[b][:], b4(gbeta), ALU.mult), reads=['vtk%d' % b, gk], writes=['vb'])
            P.op('pool', TT(e4(kbg), ktk[b][:], b4(bege[:].rearrange("p (c h) -> p c h", c=2)), ALU.mult), reads=['ktk%d' % b, 'bege'], writes=['kbg'])
            P.op('pool', TT(e4(Kd), ktk[b][:], b4(ekd[:].rearrange("p (c h) -> p c h", c=2)), ALU.mult), reads=['ktk%d' % b, 'ekd'], writes=[kKd])
            yield
            for c in range(2):
                psU, pkU = self.psum()
                for h in range(4):
                    it = c * 4 + h
                    P.op('pe', MM(psU[0:64, h * 128:(h + 1) * 128], R[:, it, :], vb[:, it, :], True, True), reads=['R', 'vb'], writes=[pkU])
                P.op('act', ACP(usb[:, 4 * c:4 * c + 4, :].rearrange("p a b -> p (a b)"), psU[0:64, :]), reads=[pkU], writes=[(kusb, c)])
            psW, pkW = self.psum()
            for it in range(8):
                sl = slice(it * 64, (it + 1) * 64)
                P.op('pe', MM(psW[:, sl], kbg[:, it, :], R[:, it, :], True, True), reads=['R', 'kbg'], writes=[pkW])
            P.op('act', ACP(f2(WT), psW[:, :]), reads=[pkW], writes=[kWT])
            yield

        def scan_gen(ti, g0, oi):
            nonlocal cur
            b = ti % 2
            want_o = oi is not None
            egt, QKm, Kd, usb, WT, QdT = egt2[b], QKm2[b], Kd2[b], usb2[b], WT2[b], QdT2[b]
            kegt, kQKm, kKd, kusb, kWT, kQdT = 'egt%d' % b, 'QKm%d' % b, 'Kd%d' % b, 'usb%d' % b, 'WT%d' % b, 'QdT%d' % b
            ob = ti % 2
            for c in ((1, 0) if rev else (0, 1)):
                So, Sn = Sst[cur], Sst[1 - cur]
                ko, kn_ = 'Sst%d' % cur, 'Sst%d' % (1 - cur)
                psS, pkS = self.psum()
                for h in range(4):
                    P.op('pe', MM(psS[0:64, h * 128:(h + 1) * 128], WT[:, 4 * c + h, :], So[:, h, :], True, True), reads=[kWT, ko], writes=[pkS])
                P.op('dve', TT(f2(vnew), usb[:, 4 * c:4 * c + 4, :].rearrange("p a b -> p (a b)"), psS[0:64, :], ALU.subtract),
                     reads=[pkS, (kusb, c)], writes=['vnew'])
                if want_o:
                    psO, pkO = self.psum()
                    for h in range(4):
                        it = 4 * c + h
                        P.op('pe', MM(psO[0:64, h * 128:(h + 1) * 128], QdT[:, it, :], So[:, h, :], True, False), reads=[kQdT, ko], writes=[pkO])
                        P.op('pe', MM(psO[0:64, h * 128:(h + 1) * 128], QKm[:, it, :], vnew[:, h, :], False, True), reads=[kQKm, 'vnew'], writes=[pkO])
                    P.op('act', ACP(osb[ob][:, c, :, :].rearrange("p a b -> p (a b)"), psO[0:64, :]), reads=[pkO], writes=[('osb', ob, c)])
                yield
                psD2, pkD2 = self.psum()
                for h in range(4):
                    P.op('pe', MM(psD2[:, h * 128:(h + 1) * 128], Kd[:, 4 * c + h, :], vnew[:, h, :], True, True), reads=[kKd, 'vnew'], writes=[pkD2])
                for h in range(4):
                    P.op('dve', STT(Sn[:, h, :], So[:, h, :], egt[:, 4 * c + h:4 * c + h + 1], psD2[:, h * 128:(h + 1) * 128], ALU.mult, ALU.add),
                         reads=[ko, kegt, pkD2], writes=[(kn_, h)] if False else [kn_])
                cur = 1 - cur
                yield
            if want_o:
                P.op('pool', DMA(out_ap[oi * 128:(oi + 1) * 128, :].rearrange("(c p) f -> p c f", p=64), osb[ob][:].rearrange("p c h e -> p c (h e)")),
                     reads=[('osb', ob, 0), ('osb', ob, 1)], writes=[(tag + 'o_s', oi)], dma=tag + 'osb%d' % ob)
            yield

        def drain(*gens):
            gens = [x for x in gens if x is not None]
            while gens:
                for x in list(gens):
                    try:
                        next(x)
                    except StopIteration:
                        gens.remove(x)
        prev = None
        for ti, (g0, oi) in enumerate(tiles):
            drain(prep_gen(ti, g0, oi), prev)
            prev = scan_gen(ti, g0, oi)
            if extra is not None:
                next(extra, None)
                next(extra, None)
        drain(prev)
        if extra is not None:
            for _ in extra:
                pass
        if 'state' in self.dbg:
            P.op('pool', DMA(self.dbg_state[tag], Sst[cur][:].rearrange("p a b -> p (a b)")), reads=['Sst%d' % cur], dma=tag + 'dbgS')
        P.barrier()
        es.close()

    def phaseG(self):
        NT, NL = self.NT, self.NL
        tilesA = [(0, None), (128, None)] + [(256 + i * 128, i) for i in range(NT)]
        esT = ExitStack()
        self.gdn_pass('ga_', 256, 0, tilesA, self.S['oA'], extra=self.t_steps(esT))
        esT.close()
        if self.stop_after == 'GA':
            return
        tilesB = [(128, None), (0, None)] + [(256 + i * 128, (i if i < NT else None)) for i in range(NL - 1, -1, -1)]
        self.gdn_pass('gb_', 512, 4, tilesB, self.S['oB'])


def _consts():
    cf = np.zeros((128, 1280), np.float32)
    cf[:, 0:128] = np.eye(128, dtype=np.float32)
    cf[:, 128:256] = 1.0
    t = np.arange(64)
    UF = (t[:, None] <= t[None, :]).astype(np.float32)
    UB = (t[:, None] >= t[None, :]).astype(np.float32)
    col = 256
    for U in (UF, UB):
        cf[0:64, col:col + 64] = U
        cf[0:64, col + 64:col + 128] = (U - 1.0) * BIG
        strict = U - np.eye(64, dtype=np.float32)
        cf[0:64, col + 128:col + 192] = (strict - 1.0) * BIG
        cf[0:64, col + 192:col + 256] = (strict.T - 1.0) * BIG
        col += 256
    cf[0:64, 768:832] = np.eye(64, dtype=np.float32)
    cf[:, 832:848] = np.arange(16, dtype=np.float32)[None, :]
    import ml_dtypes
    cb = np.zeros((128, 512), np.float32)
    cb[:, 0:128] = np.eye(128)
    k = np.arange(128)
    cb[:, 128:256] = (k[:, None] >= k[None, :])
    cb[:, 256:384] = (k[:, None] <= k[None, :])
    cb[:, 384:512] = 1.0
    return cf, cb.astype(ml_dtypes.bfloat16)


def prepare_inputs(inp, NT, cores=range(8)):
    NL = 2 * NT
    seq = NL * 128
    f32 = lambda a: np.ascontiguousarray(a, dtype=np.float32)
    w_in = inp['w_in'][0]
    qa, ka, va, dq, dk, dv, z, gb = np.split(w_in, [512, 640, 768, 1280, 1792, 2304, 2816], axis=1)

    def perm_rope(w, nh):
        w4 = w.reshape(D, nh, 2, 2, 16)
        return w4[:, :, :, ::-1, :].reshape(D, nh * 64)
    cf, cb = _consts()
    nf = 16
    freqs = (10000.0 ** (-np.arange(nf, dtype=np.float32) / nf)).astype(np.float32)
    maps = []
    for c in cores:
        b, half = c // 2, c % 2
        x = inp['x'][b, :seq]
        ctx = inp['ctx'][b]
        gbl = gb if half == 0 else gb[:, [4, 5, 6, 7, 0, 1, 2, 3, 12, 13, 14, 15, 8, 9, 10, 11]]
        conv = inp['conv_w'][0]
        alog, dtb = inp['a_log'][0], inp['dt_bias'][0]
        if half == 1:
            x, ctx, conv = x[::-1], ctx[::-1], conv[::-1]
            alog, dtb = alog[::-1], dtb[::-1]
        wext = np.concatenate([qa, perm_rope(qa, 8), ka, perm_rope(ka, 2), dq, dk, dv, z, va, gbl], axis=1)
        assert wext.shape[1] == WEXT
        cvec = np.concatenate([inp['c'][b].reshape(8, 128).T, inp['c_ctx'].reshape(8, 128).T], axis=1)
        ntab = (NT + 1) * 128
        tau = np.arange(ntab)
        pos = tau if half == 0 else (seq - 1 - tau)
        row = (pos // 64).astype(np.float32)
        colp = (pos % 64).astype(np.float32)
        ropeC = np.zeros((64, ntab), np.float32)
        ropeS = np.zeros((64, ntab), np.float32)
        for hb_, p_ in ((0, row), (32, colp)):
            ang = (p_[:, None] * freqs[None, :]).astype(np.float32)
            cs_, sn_ = np.cos(ang).astype(np.float32).T, np.sin(ang).astype(np.float32).T
            ropeC[hb_:hb_ + 16] = cs_
            ropeC[hb_ + 16:hb_ + 32] = cs_
            ropeS[hb_:hb_ + 16] = -sn_
            ropeS[hb_ + 16:hb_ + 32] = sn_
        m = {
            'x': f32(x), 'ctx': f32(ctx), 'cvec': f32(cvec),
            'w_ada': f32(inp['w_ada'][0]), 'b_ada': f32(inp['b_ada'][0].reshape(1, -1)),
            'w_in': f32(wext),
            'cw': f32(conv.reshape(5, 12, 128).transpose(2, 1, 0)),
            'adt': f32(np.concatenate([alog.reshape(-1), dtb.reshape(-1)]).reshape(1, 16)),
            'sink': f32(inp['sink'][0].reshape(1, 8)), 'wnorm': f32(np.tile(inp['dn_norm_w'][0].reshape(1, 128), (128, 4))),
            'w_out': f32(inp['w_out'][0]),
            'lnp': f32(np.tile(np.concatenate([inp['ln1_g'][0], inp['ln1_b'][0], inp['ln2_g'][0], inp['ln2_b'][0]]).reshape(1, -1), (128, 1))),
            'wq': f32(inp['peer_wq'][0]),
            'skT': f32(inp['peer_sub_keys'][0].transpose(2, 0, 1)),
            'peer_u': f32(inp['peer_u'][0]), 'peer_v': f32(inp['peer_v'][0]),
            'ropeC': ropeC, 'ropeS': ropeS, 'cf': cf, 'cb': cb,
        }
        maps.append(m)
    return maps


_NC_CACHE = {}


def kernel(**inputs):
    NT = 32
    if NT not in _NC_CACHE:
        _NC_CACHE[NT] = Builder(NT).build()
    nc = _NC_CACHE[NT]
    maps = prepare_inputs(inputs, NT)
    res = run_bass_kernel_spmd(nc, maps, core_ids=list(range(8)))
    out = np.zeros((4, 8192, D), np.float32)
    for c in range(8):
        b, half = c // 2, c % 2
        o = np.asarray(res.results[c]['out'])
        if half == 0:
            out[b, :4096] = o
        else:
            out[b, 4096:] = o[::-1]
    return out
```
